# Optimizing a Trainium2 kernel written in Bass

```python
import jax, jax.numpy as jnp
from jax import lax
import numpy as np

D_MODEL = 1024
BATCH = 8
SEQ = 4096
DEPTH = 2

CHUNK = 64
EPS = 1e-6
N_EVEN = (DEPTH + 1) // 2
N_ODD = DEPTH // 2

POOL_WINDOWS = (2, 4, 8, 16)
N_POOL_GROUPS = len(POOL_WINDOWS)
POOL_GROUP_DIM = D_MODEL // 8
POOL_WIDTH = N_POOL_GROUPS * POOL_GROUP_DIM

ATT_HEADS = 8
ATT_HEAD_DIM = 64
ATT_WIDTH = ATT_HEADS * ATT_HEAD_DIM
LEFT_CHUNKS = 8
LEFT = LEFT_CHUNKS * CHUNK
BAND = (LEFT_CHUNKS + 1) * CHUNK
MAX_REL = 128
N_REL = 2 * MAX_REL + 1
ATT_SCALE = ATT_HEAD_DIM ** -0.5
AB_IN_WIDTH = POOL_WIDTH + 3 * ATT_WIDTH
AB_MIX_WIDTH = POOL_WIDTH + ATT_WIDTH

SGU_CHUNK = 128
SGU_HEADS = 8
SGU_WIDTH = D_MODEL
SGU_HEAD_DIM = SGU_WIDTH // SGU_HEADS

N_GROUPS = 4
EXPERTS_PER_GROUP = 4
N_EXPERTS = N_GROUPS * EXPERTS_PER_GROUP
TOP_K_IN_GROUP = 2
D_EXPERT = 256

kernel_name = "hybrid_pool_chunkattn_gmlp_hmoe"


def rmsnorm(x, g):
    xf = x.astype(jnp.float32)
    y = xf * lax.rsqrt(jnp.mean(xf * xf, axis=-1, keepdims=True) + EPS) * g.astype(jnp.float32)
    return y.astype(x.dtype)


def layernorm(x, g, b):
    xf = x.astype(jnp.float32)
    mu = jnp.mean(xf, axis=-1, keepdims=True)
    var = jnp.mean(jnp.square(xf - mu), axis=-1, keepdims=True)
    y = (xf - mu) * lax.rsqrt(var + EPS) * g.astype(jnp.float32) + b.astype(jnp.float32)
    return y.astype(x.dtype)


def pool_mixer(p, w_pool, pool_scale):
    b_, s_, _ = p.shape
    pf = p.astype(jnp.float32).reshape(b_, s_, N_POOL_GROUPS, POOL_GROUP_DIM)
    cs = jnp.cumsum(pf, axis=1)
    cs = jnp.concatenate([jnp.zeros_like(cs[:, :1]), cs], axis=1)
    t = jnp.arange(s_)
    means = []
    for gi, w in enumerate(POOL_WINDOWS):
        start = jnp.maximum(t + 1 - w, 0)
        cnt = (t + 1 - start).astype(jnp.float32)
        cs_g = cs[:, :, gi]
        means.append((cs_g[:, 1:] - cs_g[:, start]) / cnt[None, :, None])
    pooled = jnp.stack(means, axis=2) - pf
    y = jnp.einsum('bsgc,gcd->bsgd', pooled, w_pool.astype(jnp.float32))
    y = y * pool_scale.astype(jnp.float32).reshape(N_POOL_GROUPS, POOL_GROUP_DIM)
    return y.reshape(b_, s_, POOL_WIDTH).astype(p.dtype)


def chunk_attention(q, k, v, rel_bias):
    b_, s_, h_, d_ = q.shape
    nc = s_ // CHUNK
    kp = jnp.pad(k, ((0, 0), (LEFT, 0), (0, 0), (0, 0)))
    vp = jnp.pad(v, ((0, 0), (LEFT, 0), (0, 0), (0, 0)))
    q_blocks = q.reshape(b_, nc, CHUNK, h_, d_).transpose(1, 0, 2, 3, 4)
    rel = jnp.arange(BAND)[None, :] - LEFT - jnp.arange(CHUNK)[:, None]
    bias = rel_bias.astype(jnp.float32)[:, jnp.clip(rel, -MAX_REL, MAX_REL) + MAX_REL]
    band_off = jnp.arange(BAND) - LEFT

    def one_chunk(args):
        c, qb = args
        kb = lax.dynamic_slice_in_dim(kp, c * CHUNK, BAND, axis=1)
        vb = lax.dynamic_slice_in_dim(vp, c * CHUNK, BAND, axis=1)
        s = jnp.einsum('bqhd,bkhd->bhqk', qb, kb).astype(jnp.float32) * ATT_SCALE + bias[None]
        valid = (c * CHUNK + band_off) >= 0
        s = jnp.where(valid[None, None, None, :], s, -1e30)
        pr = jax.nn.softmax(s, axis=-1)
        return jnp.einsum('bhqk,bkhd->bqhd', pr.astype(vb.dtype), vb)

    o = lax.map(one_chunk, (jnp.arange(nc), q_blocks))
    return o.transpose(1, 0, 2, 3, 4).reshape(b_, s_, h_ * d_)


def pool_attn_mixer(h, w_in, w_pool, pool_scale, rel_bias, w_out):
    b_, s_, _ = h.shape
    z = h @ w_in
    p = z[..., :POOL_WIDTH]
    q, k, v = jnp.split(z[..., POOL_WIDTH:], 3, axis=-1)
    shp = (b_, s_, ATT_HEADS, ATT_HEAD_DIM)
    a_out = pool_mixer(p, w_pool, pool_scale)
    b_out = chunk_attention(q.reshape(shp), k.reshape(shp), v.reshape(shp), rel_bias)
    return jnp.concatenate([a_out.astype(h.dtype), b_out.astype(h.dtype)], axis=-1) @ w_out


def sgu_mixer(h, w_in, b_in, ln_g, ln_b, w_s, b_s, w_out):
    b_, s_, _ = h.shape
    z = jax.nn.gelu(h @ w_in + b_in, approximate=False)
    u, v = jnp.split(z, 2, axis=-1)
    v = layernorm(v, ln_g, ln_b)
    nch = s_ // SGU_CHUNK
    vc = v.reshape(b_, nch, SGU_CHUNK, SGU_HEADS, SGU_HEAD_DIM)
    causal = jnp.tril(jnp.ones((SGU_CHUNK, SGU_CHUNK), dtype=w_s.dtype))
    ws = w_s * causal[None]
    mixed = jnp.einsum('hts,bnshd->bnthd', ws, vc) + b_s.T[None, None, :, :, None]
    return (u * mixed.reshape(b_, s_, SGU_WIDTH)) @ w_out


def hier_moe(h, wg_router, bg_router, we_router, be_router, w_gate, w_up, w_down):
    b_, s_, d_ = h.shape
    t = h.reshape(-1, d_)
    n = t.shape[0]
    g_logits = (t @ wg_router).astype(jnp.float32) + bg_router.astype(jnp.float32)
    g_prob = jax.nn.softmax(g_logits, axis=-1)
    g_top, g_idx = lax.top_k(g_logits, 1)
    g_w = jnp.take_along_axis(g_prob, g_idx, axis=-1)[:, 0]
    e_logits = jnp.einsum('nd,gde->nge', t, we_router).astype(jnp.float32) + be_router.astype(jnp.float32)
    e_sel = jnp.take_along_axis(e_logits, g_idx[:, :, None], axis=1)[:, 0]
    e_top, e_idx = lax.top_k(e_sel, TOP_K_IN_GROUP)
    e_w = jax.nn.softmax(e_top, axis=-1)
    expert_id = g_idx * EXPERTS_PER_GROUP + e_idx
    combine = jnp.sum(jax.nn.one_hot(expert_id, N_EXPERTS, dtype=jnp.float32)
                      * (g_w[:, None] * e_w)[..., None], axis=1)
    hg = jnp.einsum('nd,edf->nef', t, w_gate)
    hu = jnp.einsum('nd,edf->nef', t, w_up)
    act = jax.nn.silu(hg) * hu * combine.astype(t.dtype)[..., None]
    out = jnp.einsum('nef,efd->nd', act, w_down)
    return out.reshape(b_, s_, d_)


def setup_inputs(seed: int = 0) -> dict:
    key = jax.random.key(seed)
    ks = jax.random.split(key, 24)
    nrm = lambda k, shp: jax.random.normal(k, shp, dtype=jnp.float32)
    D = D_MODEL
    return {
        "x": nrm(ks[0], (BATCH, SEQ, D)),
        "norm_mix_g": 1.0 + 0.1 * nrm(ks[1], (DEPTH, D)),
        "norm_ffn_g": 1.0 + 0.1 * nrm(ks[2], (DEPTH, D)),
        "final_norm_g": 1.0 + 0.1 * nrm(ks[3], (D,)),
        "ab_w_in": nrm(ks[4], (N_EVEN, D, AB_IN_WIDTH)) * D ** -0.5,
        "pool_w": nrm(ks[5], (N_EVEN, N_POOL_GROUPS, POOL_GROUP_DIM, POOL_GROUP_DIM)) * POOL_GROUP_DIM ** -0.5,
        "pool_scale": 1.0 + 0.1 * nrm(ks[6], (N_EVEN, POOL_WIDTH)),
        "att_rel_bias": 0.1 * nrm(ks[7], (N_EVEN, ATT_HEADS, N_REL)),
        "ab_w_out": nrm(ks[8], (N_EVEN, AB_MIX_WIDTH, D)) * AB_MIX_WIDTH ** -0.5,
        "sgu_w_in": nrm(ks[9], (N_ODD, D, 2 * SGU_WIDTH)) * D ** -0.5,
        "sgu_b_in": 0.02 * nrm(ks[10], (N_ODD, 2 * SGU_WIDTH)),
        "sgu_ln_g": 1.0 + 0.1 * nrm(ks[11], (N_ODD, SGU_WIDTH)),
        "sgu_ln_b": 0.02 * nrm(ks[12], (N_ODD, SGU_WIDTH)),
        "sgu_w_s": nrm(ks[13], (N_ODD, SGU_HEADS, SGU_CHUNK, SGU_CHUNK)) * SGU_CHUNK ** -0.5,
        "sgu_b_s": 1.0 + 0.1 * nrm(ks[14], (N_ODD, SGU_HEADS, SGU_CHUNK)),
        "sgu_w_out": nrm(ks[15], (N_ODD, SGU_WIDTH, D)) * SGU_WIDTH ** -0.5,
        "moe_wg_router": nrm(ks[16], (DEPTH, D, N_GROUPS)) * D ** -0.5,
        "moe_bg_router": 0.01 * nrm(ks[17], (DEPTH, N_GROUPS)),
        "moe_we_router": nrm(ks[18], (DEPTH, N_GROUPS, D, EXPERTS_PER_GROUP)) * D ** -0.5,
        "moe_be_router": 0.01 * nrm(ks[19], (DEPTH, N_GROUPS, EXPERTS_PER_GROUP)),
        "moe_w_gate": nrm(ks[20], (DEPTH, N_EXPERTS, D, D_EXPERT)) * D ** -0.5,
        "moe_w_up": nrm(ks[21], (DEPTH, N_EXPERTS, D, D_EXPERT)) * D ** -0.5,
        "moe_w_down": nrm(ks[22], (DEPTH, N_EXPERTS, D_EXPERT, D)) * D_EXPERT ** -0.5,
    }


def reference(x, norm_mix_g, norm_ffn_g, final_norm_g,
              ab_w_in, pool_w, pool_scale, att_rel_bias, ab_w_out,
              sgu_w_in, sgu_b_in, sgu_ln_g, sgu_ln_b, sgu_w_s, sgu_b_s, sgu_w_out,
              moe_wg_router, moe_bg_router, moe_we_router, moe_be_router,
              moe_w_gate, moe_w_up, moe_w_down):
    h = x
    for layer in range(DEPTH):
        i = layer // 2
        y = rmsnorm(h, norm_mix_g[layer])
        if layer % 2 == 0:
            y = pool_attn_mixer(y, ab_w_in[i], pool_w[i], pool_scale[i], att_rel_bias[i], ab_w_out[i])
        else:
            y = sgu_mixer(y, sgu_w_in[i], sgu_b_in[i], sgu_ln_g[i], sgu_ln_b[i],
                          sgu_w_s[i], sgu_b_s[i], sgu_w_out[i])
        h = h + y
        y = hier_moe(rmsnorm(h, norm_ffn_g[layer]), moe_wg_router[layer], moe_bg_router[layer],
                     moe_we_router[layer], moe_be_router[layer],
                     moe_w_gate[layer], moe_w_up[layer], moe_w_down[layer])
        h = h + y
    return rmsnorm(h, final_norm_g)
```

```python
import numpy as np
from contextlib import ExitStack
import concourse.bass as bass
import concourse.mybir as mybir
from concourse.bass_utils import run_bass_kernel_spmd

F32 = mybir.dt.float32
BF16 = mybir.dt.bfloat16
AF = mybir.ActivationFunctionType
ALU = mybir.AluOpType
AX = mybir.AxisListType

D = 1024
SEQ = 4096
TS = 1024
NT = TS // 512
NBLK = TS // 128
EPS = 1e-6
ATT_SCALE = 64 ** -0.5
ENGS = ["sync", "act", "pool", "dve", "pe"]


class Op:
    __slots__ = ("eng", "fn", "deps", "sig", "sigidx", "grp", "is_dma")

    def __init__(self, eng, fn, is_dma=False, grp=None):
        self.eng = eng; self.fn = fn; self.deps = set(); self.sig = False
        self.sigidx = None; self.grp = grp; self.is_dma = is_dma


class Grp:
    def __init__(self, sem):
        self.sem = sem; self.val = 0


class Prog:
    def __init__(self):
        self.ops = {e: [] for e in ENGS}
        self.tiles = {}
        self.dma_cnt = {}
        self.pending = {e: set() for e in ENGS}
        self.final_tokens = []

    def new_group(self, sem):
        g = Grp(sem)
        g.val = self.dma_cnt.get(sem, 0)
        return g

    def add(self, eng, fn, reads=(), writes=(), grp=None):
        op = Op(eng, fn, is_dma=grp is not None, grp=grp)
        if grp is not None:
            self.dma_cnt[grp.sem] = self.dma_cnt.get(grp.sem, 0) + 16
            grp.val = self.dma_cnt[grp.sem]
        raw = set(); oth = set()
        for k in reads:
            st = self.tiles.get(k)
            if st is not None and st[0] is not None:
                raw.add(st[0])
        for k in writes:
            st = self.tiles.get(k)
            if st is not None:
                if st[0] is not None:
                    oth.add(st[0])
                oth.update(st[1].values())
        for d in raw | oth:
            if d is op:
                continue
            if d.grp is not None and d.grp is op.grp:
                continue
            if d.is_dma or op.is_dma or d.eng != eng:
                op.deps.add(d)
            elif eng != "pe":
                op.deps.add(d)
        for d in self.pending[eng]:
            op.deps.add(d)
        self.pending[eng] = set()
        for k in reads:
            self.tiles.setdefault(k, [None, {}])[1][eng if not op.is_dma else id(op)] = op
        for k in writes:
            self.tiles[k] = [op, {}]
        self.ops[eng].append(op)
        return op

    def barrier(self, engs=("sync", "act", "dve", "pe")):
        lasts = {e: self.ops[e][-1] for e in engs if self.ops[e]}
        n0 = getattr(self, "_sync_mark", 0)
        dmas = [o for o in self.ops["sync"][n0:] if o.is_dma]
        self._sync_mark = len(self.ops["sync"])
        for e in engs:
            for e2, o in lasts.items():
                if e2 != e:
                    self.pending[e].add(o)
            for o in dmas:
                self.pending[e].add(o)

    def emit(self, nc, es):
        for e in ENGS:
            for op in self.ops[e]:
                for d in op.deps:
                    if not d.is_dma:
                        d.sig = True
        for e in ENGS:
            n = 0
            for op in self.ops[e]:
                if op.sig and not op.is_dma:
                    n += 1; op.sigidx = n
        sems = {e: es.enter_context(nc.semaphore("s_" + e)) for e in ENGS}
        dsems = {}
        for e in ENGS:
            for op in self.ops[e]:
                if op.is_dma and op.grp.sem not in dsems:
                    dsems[op.grp.sem] = es.enter_context(nc.semaphore("d_" + op.grp.sem))
        block = es.enter_context(nc.Block())
        final_tokens = self.final_tokens

        def run(e, eng):
            waited = {}
            for op in self.ops[e]:
                need = {}
                for d in op.deps:
                    if d.is_dma:
                        s, v = dsems[d.grp.sem], d.grp.val
                    else:
                        s, v = sems[d.eng], d.sigidx
                    key = id(s)
                    if waited.get(key, 0) < v:
                        if key not in need or need[key][1] < v:
                            need[key] = (s, v)
                for key, (s, v) in need.items():
                    eng.wait_ge(s, v); waited[key] = v
                inst = op.fn(eng)
                if op.is_dma:
                    inst.then_inc(dsems[op.grp.sem], 16)
                elif op.sig:
                    inst.then_inc(sems[e], 1)
            if e == "sync":
                for g in final_tokens:
                    eng.wait_ge(dsems[g.sem], g.val)

        block.sync(lambda eng: run("sync", eng))
        block.scalar(lambda eng: run("act", eng))
        block.gpsimd(lambda eng: run("pool", eng))
        block.vector(lambda eng: run("dve", eng))
        block.tensor(lambda eng: run("pe", eng))


FAST_RECIP = False


def RECIP(e, out, in_):
    if FAST_RECIP:
        return e.reciprocal_approx_fast(out=out, in_=in_)
    return e.reciprocal(out=out, in_=in_)


def build_program(n_super=SEQ // TS, stop_stage=99):
    nc = bass.Bass("TRN2", target_bir_lowering=False)
    P = Prog()

    def din(name, shape):
        return nc.dram_tensor(name, list(shape), F32, kind="ExternalInput").ap()

    x = din("x", [SEQ, D])
    out = nc.dram_tensor("out", [SEQ, D], F32, kind="ExternalOutput").ap()
    gains_d = din("gains", [128, 5 * 8])
    ab_w_in = din("ab_w_in", [D, 2048]); pool_w = din("pool_w", [4, 128, 128])
    pscale_d = din("pscale", [128, 4]); btab_d = din("btab", [128, 8 * 256]); r0tab_d = din("r0tab", [128, 8 * 256])
    emask_d = din("emask", [128, 256]); ab_w_out = din("ab_w_out", [D, D])
    sgu_w_in = din("sgu_w_in", [D, 2048]); bu_d = din("bu", [128, 8]); bv_d = din("bv", [1024])
    lnfm_d = din("lnfm", [128, 16]); wsT_d = din("wsT", [128, 1024])
    trimask_d = din("trimask", [128, 128]); bs_d = din("bs", [1024]); sgu_w_out = din("sgu_w_out", [D, D])
    wr_d = din("wr", [2, D, 20]); rb_d = din("rbias", [40])
    w_gate = din("w_gate", [2, 16, D, 256]); w_up = din("w_up", [2, 16, D, 256]); w_down = din("w_down", [2, 16, 256, D])
    ident_d = din("ident", [128, 128]); sele_d = din("sele", [32, 16 * 128]); invcnt_d = din("invcnt", [128, 64])

    es = ExitStack()
    with es:
        avail = (nc.sbuf_bytes_remaining // 256) * 256 - 256
        arena = es.enter_context(nc.sbuf_tensor("arena", [128, avail // 4], F32))
        psum = es.enter_context(nc.psum_tensor("psum", [128, 4096], F32))
        state = {"off": 0}

        def alloc(shape, dtype=F32, parts=None):
            n = int(np.prod(shape[1:]))
            nbytes = n * (4 if dtype == F32 else 2)
            nbytes = (nbytes + 63) // 64 * 64
            o = state["off"]; state["off"] += nbytes
            assert state["off"] <= avail, ("SBUF overflow", state["off"], avail)
            v = arena[:, o // 4:(o + nbytes) // 4]
            if dtype != F32:
                v = v.bitcast(dtype)
            v = v[:, 0:n]
            if len(shape) == 3:
                v = v.rearrange("p (a b) -> p a b", a=shape[1])
            elif len(shape) == 4:
                v = v.rearrange("p (a b c) -> p a b c", a=shape[1], b=shape[2])
            return v

        def bank(b, n=1):
            return psum[:, b * 512:(b + n) * 512]

        rr = {"b": 0}

        def nextbank():
            b = rr["b"]; rr["b"] = (b + 1) % 8
            return b

        h = alloc([128, 8, TS])
        NSLOT = 6
        ring = [alloc([128, 4096], BF16) for _ in range(NSLOT)]
        ktail = alloc([128, 4, 512], BF16); vtail = alloc([128, 4, 8, 128], BF16); ptail = alloc([128, 4, 16])
        etab = alloc([128, 8, 256])
        ident = alloc([128, 128]); ones_bf = alloc([128, 128], BF16); gains = alloc([128, 5, 8])
        pscale = alloc([128, 4]); b_u = alloc([128, 8]); epst = alloc([128, 1])
        sele = alloc([128, 16, 128], BF16); poolw = alloc([128, 4, 128], BF16); invcnt = alloc([128, 4, 16])
        wr32 = alloc([128, 2, 8, 20]); rbias = alloc([128, 2, 20])
        sqp = [alloc([128, 512], BF16) for _ in range(4)]; rstdp = [alloc([128, 512]) for _ in range(NT)]
        arena_base = state["off"]
        XS_OFF = 57344
        state["off"] = arena_base + XS_OFF
        xs = [alloc([128, D]) for _ in range(4)]
        assert state["off"] <= arena_base + 73728
        state["off"] = arena_base

        ring_state = {"n": 0}

        def ring_load(parts):
            i = ring_state["n"] % NSLOT; ring_state["n"] += 1
            g = P.new_group("ring%d" % i)
            for dst_fn, src in parts:
                dst = dst_fn(ring[i])
                P.add("pool", lambda e, dst=dst, src=src: e.dma_start(out=dst, in_=src), writes=[("ring", i)], grp=g)
            return i

        gc = P.new_group("const")

        gcp = P.new_group("constp")

        def cload(dst, src, eng="sync", key=None):
            P.add(eng, lambda e, dst=dst, src=src: e.dma_start(out=dst, in_=src), writes=[key], grp=(gc if eng == "sync" else gcp))

        cload(ident, ident_d[:, :], key="ident")
        cload(gains.rearrange("p a b -> p (a b)"), gains_d[:, :], key="gains")
        cload(pscale, pscale_d[:, :], key="pscale")
        cload(b_u, bu_d[:, :], key="b_u")
        cload(invcnt.rearrange("p a b -> p (a b)"), invcnt_d[:, :], key="invcnt")
        cload(wr32.rearrange("p l k n -> p l (k n)")[:, 0, :], wr_d[0].rearrange("(p kc) n -> p (kc n)", kc=8), key="wr32")
        cload(wr32.rearrange("p l k n -> p l (k n)")[:, 1, :], wr_d[1].rearrange("(p kc) n -> p (kc n)", kc=8), key="wr32")
        cload(rbias.rearrange("p a b -> p (a b)"), rb_d.partition_broadcast(128), key="rbias")
        cload(sele.rearrange("p a b -> p (a b)")[0:32, :], sele_d[:, :], eng="pool", key="sele")
        cload(poolw, pool_w.rearrange("g c d -> c g d"), eng="pool", key="poolw")
        state["off"] = arena_base
        btab = alloc([128, 8, 256]); r0tab = alloc([128, 8, 256]); emask = alloc([128, 256])
        cload(btab.rearrange("p a b -> p (a b)"), btab_d[:, :], key="btab")
        cload(r0tab.rearrange("p a b -> p (a b)"), r0tab_d[:, :], key="r0tab")
        cload(emask, emask_d[:, :], key="emask")
        P.add("dve", lambda e: e.memset(ones_bf, 1.0), writes=["ones"])
        P.add("dve", lambda e: e.memset(epst, EPS), writes=["eps"])
        P.add("dve", lambda e: e.memset(vtail.rearrange("p a b c -> p (a b c)"), 1.0), writes=["vtail"])
        P.add("dve", lambda e: e.tensor_tensor(out=btab, in0=btab, in1=r0tab, op=ALU.subtract),
              reads=["btab", "r0tab"], writes=["btab"])
        P.add("act", lambda e: e.activation(out=btab, in_=btab, func=AF.Exp), reads=["btab"], writes=["btab"])
        P.add("dve", lambda e: e.tensor_tensor(out=etab, in0=btab, in1=emask.unsqueeze(1).broadcast_to([128, 8, 256]),
                                               op=ALU.mult), reads=["btab", "emask"], writes=["etab"])
        for l in range(2):
            for kc in range(8):
                P.add("dve", lambda e, l=l, kc=kc: e.tensor_scalar(out=wr32[:, l, kc, :], in0=wr32[:, l, kc, :],
                                                                  scalar1=gains[:, 1 + 2 * l, kc:kc + 1], scalar2=None, op0=ALU.mult),
                      reads=["wr32", "gains"], writes=["wr32"])
        P.barrier()

        HK = lambda kc, tt: ("h", kc, tt)
        YK = lambda kc, tt: ("y", kc, tt)

        def norm_stats(tt):
            ts_ = slice(tt * 512, (tt + 1) * 512)
            b = nextbank()
            for kc in range(8):
                P.add("act", lambda e, kc=kc, ts_=ts_: e.activation(out=sqp[kc % 4], in_=h[:, kc, ts_], func=AF.Square),
                      reads=[HK(kc, tt)], writes=[("sq", kc % 4)])
                P.add("pe", lambda e, kc=kc, b=b: e.matmul(bank(b), lhsT=ones_bf, rhs=sqp[kc % 4], start=(kc == 0), stop=(kc == 7)),
                      reads=[("sq", kc % 4), "ones"], writes=[("ps", b)])
            P.add("act", lambda e, b=b, tt=tt: e.activation(out=rstdp[tt], in_=bank(b), func=AF.Ln, scale=1.0 / D, bias=epst[:, 0:1]),
                  reads=[("ps", b), "eps"], writes=[("rstd", tt)])
            P.add("act", lambda e, tt=tt: e.activation(out=rstdp[tt], in_=rstdp[tt], func=AF.Exp, scale=-0.5),
                  reads=[("rstd", tt)], writes=[("rstd", tt)])

        def rmsnorm(gi, ym, out_dtype_note=None):
            for tt in range(NT):
                ts_ = slice(tt * 512, (tt + 1) * 512)
                for kc in range(8):
                    P.add("dve", lambda e, kc=kc, ts_=ts_, tt=tt: e.scalar_tensor_tensor(
                        out=ym[:, kc, ts_], in0=h[:, kc, ts_], scalar=gains[:, gi, kc:kc + 1], in1=rstdp[tt],
                        op0=ALU.mult, op1=ALU.mult), reads=[HK(kc, tt), ("rstd", tt), "gains"], writes=[YK(kc, tt)])
            return rstdp

        def proj_to_h(wslots, src):
            for tt in range(NT):
                ts_ = slice(tt * 512, (tt + 1) * 512)
                for kco in range(8):
                    b = nextbank()
                    for kc in range(8):
                        w = ring[wslots[kc // 4]].rearrange("p (a b) -> p a b", a=4)
                        P.add("pe", lambda e, w=w, kc=kc, kco=kco, b=b, ts_=ts_: e.matmul(
                            bank(b), lhsT=w[:, kc % 4, kco:1024:8], rhs=src[:, kc, ts_], start=(kc == 0), stop=(kc == 7)),
                            reads=[("ring", wslots[kc // 4]), YK(kc, tt)], writes=[("ps", b)])
                    P.add("dve", lambda e, kco=kco, b=b, ts_=ts_: e.tensor_tensor(out=h[:, kco, ts_], in0=bank(b), in1=h[:, kco, ts_], op=ALU.add),
                          reads=[("ps", b), HK(kco, tt)], writes=[HK(kco, tt)])
                norm_stats(tt)

        def load_w_out(wd):
            s = []
            for half in range(2):
                src = wd.rearrange("(kc p) n -> p kc n", p=128)[:, half * 4:(half + 1) * 4, :]
                s.append(ring_load([(lambda r: r.rearrange("p (a b) -> p a b", a=4), src)]))
            return s

        def load_w_in(wd, order=(0, 1, 2, 3)):
            s = [None] * 4
            for q in order:
                src = wd.rearrange("(p kc) n -> p kc n", kc=8)[:, :, q * 512:(q + 1) * 512]
                s[q] = ring_load([(lambda r: r.rearrange("p (a b) -> p a b", a=8), src)])
            return s

        SGU_C_OFF = 73728

        def sgu_const_views():
            o = state["off"]; state["off"] = arena_base + SGU_C_OFF
            v = (alloc([128, 1024]), alloc([128, 8, 128]), alloc([128, 8, 128]), alloc([128, 128]), alloc([128, 2, 8]))
            state["off"] = o
            return v

        def sgu_const_load():
            bvB, bsB, wsT32, trim, lnfm = sgu_const_views()
            gsc = P.new_group("sguc")
            for dst, src, k in ((bvB, bv_d.partition_broadcast(128), "bvB"),
                                (bsB.rearrange("p a b -> p (a b)"), bs_d.partition_broadcast(128), "bsB"),
                                (wsT32.rearrange("p a b -> p (a b)"), wsT_d[:, :], "wsT32"), (trim, trimask_d[:, :], "trim"),
                                (lnfm.rearrange("p a b -> p (a b)"), lnfm_d[:, :], "lnfm")):
                P.add("sync", lambda e, dst=dst, src=src: e.dma_start(out=dst, in_=src), writes=[k], grp=gsc)

        def moe_phase(l):
            state["off"] = arena_base
            if l == 0:
                sgu_const_load()
            tT = alloc([128, 8, TS], BF16)
            rstd = rmsnorm(1 + 2 * l, tT)
            Lb = nextbank(); Tb = nextbank()
            for blk in range(NBLK):
                tt = blk // 4
                for kc in range(8):
                    P.add("pe", lambda e, blk=blk, kc=kc: e.matmul(bank(Lb)[:, blk * 20:(blk + 1) * 20], lhsT=h[:, kc, blk * 128:(blk + 1) * 128],
                                                                  rhs=wr32[:, l, kc, :], start=(kc == 0), stop=(kc == 7)),
                          reads=[HK(kc, tt), "wr32"], writes=[("ps", Lb)])
                P.add("pe", lambda e, blk=blk, tt=tt: e.transpose(bank(Tb)[:, blk * 32:(blk + 1) * 32], rstd[tt][0:32, (blk % 4) * 128:(blk % 4 + 1) * 128], ident[0:32, 0:32]),
                      reads=[("rstd", tt), "ident"], writes=[("ps", Tb)])
            Ls = alloc([128, 8, 20]); rt = alloc([128, 8]); t84 = [alloc([128, 8, 4]) for _ in range(6)]; t8 = [alloc([128, 8]) for _ in range(6)]
            t844 = alloc([128, 8, 4, 4]); chl = alloc([128, 8, 32]); cbf = alloc([128, 8, 16], BF16); cT = alloc([128, TS], BF16)
            R = "rt_scratch"

            def dv(fn, reads=(), writes=()):
                P.add("dve", fn, reads=[R] + list(reads), writes=[R] + list(writes))

            def bc(a):
                return a.unsqueeze(2).broadcast_to([128, 8, 4])

            dv(lambda e: e.tensor_copy(rt, bank(Tb)[:, 0:256:32]), reads=[("ps", Tb)])
            dv(lambda e: e.tensor_tensor(out=Ls, in0=bank(Lb)[:, 0:160].rearrange("p (a b) -> p a b", a=8),
                                         in1=rt.unsqueeze(2).broadcast_to([128, 8, 20]), op=ALU.mult), reads=[("ps", Lb)])
            dv(lambda e: e.tensor_tensor(out=Ls, in0=Ls, in1=rbias[:, l, :].unsqueeze(1).broadcast_to([128, 8, 20]), op=ALU.add), reads=["rbias"])
            gl = Ls[:, :, 0:4]
            gmax, gs, gw, m1, m2, es_ = t8
            oh, gd, esel, e2, sel, en = t84
            dv(lambda e: e.tensor_reduce(out=gmax, in_=gl, axis=AX.X, op=ALU.max))
            dv(lambda e: e.tensor_tensor(out=oh, in0=gl, in1=bc(gmax), op=ALU.is_ge))
            dv(lambda e: e.tensor_tensor(out=gd, in0=gl, in1=bc(gmax), op=ALU.subtract))
            P.add("act", lambda e: e.activation(out=gd, in_=gd, func=AF.Exp), reads=[R], writes=[R])
            dv(lambda e: e.tensor_reduce(out=gs, in_=gd, axis=AX.X, op=ALU.add))
            dv(lambda e: e.reciprocal(out=gw, in_=gs))
            dv(lambda e: e.tensor_tensor(out=t844, in0=Ls[:, :, 4:20].rearrange("p a (g x) -> p a g x", g=4),
                                         in1=oh.unsqueeze(3).broadcast_to([128, 8, 4, 4]), op=ALU.mult))
            dv(lambda e: e.tensor_reduce(out=esel, in_=t844.rearrange("p a g x -> p a x g"), axis=AX.X, op=ALU.add))
            dv(lambda e: e.tensor_reduce(out=m1, in_=esel, axis=AX.X, op=ALU.max))
            dv(lambda e: e.tensor_tensor(out=sel, in0=esel, in1=bc(m1), op=ALU.is_ge))
            dv(lambda e: e.scalar_tensor_tensor(out=e2, in0=sel, scalar=-1e30, in1=esel, op0=ALU.mult, op1=ALU.add))
            dv(lambda e: e.tensor_reduce(out=m2, in_=e2, axis=AX.X, op=ALU.max))
            dv(lambda e: e.tensor_tensor(out=sel, in0=esel, in1=bc(m2), op=ALU.is_ge))
            dv(lambda e: e.tensor_tensor(out=e2, in0=esel, in1=bc(m1), op=ALU.subtract))
            P.add("act", lambda e: e.activation(out=e2, in_=e2, func=AF.Exp), reads=[R], writes=[R])
            dv(lambda e: e.tensor_tensor(out=en, in0=e2, in1=sel, op=ALU.mult))
            dv(lambda e: e.tensor_reduce(out=es_, in_=en, axis=AX.X, op=ALU.add))
            dv(lambda e: e.reciprocal(out=es_, in_=es_))
            dv(lambda e: e.tensor_tensor(out=es_, in0=es_, in1=gw, op=ALU.mult))
            dv(lambda e: e.tensor_tensor(out=en, in0=en, in1=bc(es_), op=ALU.mult))
            dv(lambda e: e.tensor_tensor(out=t844, in0=oh.unsqueeze(3).broadcast_to([128, 8, 4, 4]),
                                         in1=en.unsqueeze(2).broadcast_to([128, 8, 4, 4]), op=ALU.mult))
            comb = t844.rearrange("p a g x -> p a (g x)")
            dv(lambda e: e.tensor_copy(cbf, comb))
            dv(lambda e: e.tensor_copy(chl[:, :, 0:16], cbf))
            dv(lambda e: e.tensor_tensor(out=chl[:, :, 16:32], in0=comb, in1=chl[:, :, 0:16], op=ALU.subtract))
            def emit_cT():
                for half in range(2):
                    cb_ = nextbank()
                    for b4 in range(4):
                        blk = half * 4 + b4
                        P.add("pe", lambda e, blk=blk, b4=b4, cb_=cb_: e.transpose(bank(cb_)[0:32, b4 * 128:(b4 + 1) * 128], chl[:, blk, :], ident),
                              reads=[R, "ident"], writes=[("ps", cb_)])
                    P.add("act", lambda e, half=half, cb_=cb_: e.activation(out=cT[0:32, half * 512:(half + 1) * 512], in_=bank(cb_)[0:32, :], func=AF.Copy),
                          reads=[("ps", cb_)], writes=[("cT", half)])
            ct_done = [False]
            ssb = [alloc([128, 512]) for _ in range(4)]; cbs = [alloc([128, 512]) for _ in range(2)]
            actT = [alloc([128, 4, 512], BF16) for _ in range(2)]
            cnt = {"s": 0, "c": 0, "a": 0}
            slots = {}

            def pair_loads(pi):
                sl = []
                for el in range(2):
                    e_ = 2 * pi + el
                    sl.append(ring_load([
                        (lambda r: r[:, 0:2048], w_gate[l, e_].rearrange("(p kc) f -> p (kc f)", kc=8)),
                        (lambda r: r[:, 2048:4096], w_up[l, e_].rearrange("(p kc) f -> p (kc f)", kc=8))]))
                sd = ring_load([
                    (lambda r: r[:, 0:2048].rearrange("p (a b) -> p a b", a=2), w_down[l, 2 * pi].rearrange("(fc p) d -> p fc d", p=128)),
                    (lambda r: r[:, 2048:4096].rearrange("p (a b) -> p a b", a=2), w_down[l, 2 * pi + 1].rearrange("(fc p) d -> p fc d", p=128))])
                slots[pi] = (sl, sd)

            def emit_E(pi, tt, el):
                sl, sd = slots[pi]
                ts_ = slice(tt * 512, (tt + 1) * 512)
                ai = (pi * NT + tt) % 2
                e_ = 2 * pi + el
                wgu = ring[sl[el]].rearrange("p (g k f) -> p g k f", g=2, k=8)
                ci = cnt["c"] % 2; cnt["c"] += 1
                early = ct_done[0]

                def emit_cb(e_=e_, ci=ci, ts_=ts_, tt=tt):
                    bcb = nextbank()
                    P.add("pe", lambda e, bcb=bcb: e.matmul(bank(bcb), lhsT=sele[0:32, e_, :], rhs=cT[0:32, ts_], start=True, stop=True),
                          reads=["sele", ("cT", tt)], writes=[("ps", bcb)])
                    P.add("act", lambda e, bcb=bcb: e.activation(out=cbs[ci], in_=bank(bcb), func=AF.Copy),
                          reads=[("ps", bcb)], writes=[("cbs", ci)])

                def emit_mults(fc, si, bu_, ci=ci, ai=ai, el=el):
                    P.add("dve", lambda e: e.tensor_tensor(out=ssb[si], in0=ssb[si], in1=cbs[ci], op=ALU.mult),
                          reads=[("ssb", si), ("cbs", ci)], writes=[("ssb", si)])
                    P.add("dve", lambda e: e.tensor_tensor(out=actT[ai][:, el * 2 + fc, :], in0=bank(bu_), in1=ssb[si], op=ALU.mult),
                          reads=[("ps", bu_), ("ssb", si)], writes=[("actT", ai)])

                if early:
                    emit_cb()
                per_fc = []
                for fc in range(2):
                    bg = nextbank(); bu_ = nextbank(); si = cnt["s"] % 4; cnt["s"] += 1
                    for gi_, bb in ((0, bg), (1, bu_)):
                        for kc in range(8):
                            P.add("pe", lambda e, wgu=wgu, gi_=gi_, kc=kc, fc=fc, bb=bb, ts_=ts_: e.matmul(
                                bank(bb), lhsT=wgu[:, gi_, kc, fc * 128:(fc + 1) * 128], rhs=tT[:, kc, ts_], start=(kc == 0), stop=(kc == 7)),
                                reads=[("ring", sl[el]), YK(kc, tt)], writes=[("ps", bb)])
                    P.add("act", lambda e, bg=bg, si=si: e.activation(out=ssb[si], in_=bank(bg), func=AF.Silu),
                          reads=[("ps", bg)], writes=[("ssb", si)])
                    if early:
                        emit_mults(fc, si, bu_)
                    else:
                        per_fc.append((fc, si, bu_))
                if not early:
                    emit_cT(); ct_done[0] = True
                    emit_cb()
                    for fc, si, bu_ in per_fc:
                        emit_mults(fc, si, bu_)

            def emit_D(pi, tt):
                sl, sd = slots[pi]
                ts_ = slice(tt * 512, (tt + 1) * 512)
                ai = (pi * NT + tt) % 2
                for kco in range(8):
                    b = nextbank()
                    for el in range(2):
                        wd = ring[sd][:, el * 2048:(el + 1) * 2048].rearrange("p (a b) -> p a b", a=2)
                        for fc in range(2):
                            first = (el == 0 and fc == 0); last = (el == 1 and fc == 1)
                            P.add("pe", lambda e, wd=wd, fc=fc, kco=kco, b=b, ai=ai, el=el, first=first, last=last: e.matmul(
                                bank(b), lhsT=wd[:, fc, kco:1024:8], rhs=actT[ai][:, el * 2 + fc, :], start=first, stop=last),
                                reads=[("ring", sd), ("actT", ai)], writes=[("ps", b)])
                    P.add("dve", lambda e, kco=kco, b=b, ts_=ts_: e.tensor_tensor(out=h[:, kco, ts_], in0=bank(b), in1=h[:, kco, ts_], op=ALU.add),
                          reads=[("ps", b), HK(kco, tt)], writes=[HK(kco, tt)])
                if pi == 7:
                    norm_stats(tt)

            assert state["off"] <= arena_base + XS_OFF, state["off"] - arena_base
            pts = [(pi, tt) for pi in range(8) for tt in range(NT)]
            pair_loads(0)
            emit_E(0, 0, 0)
            for k, (pi, tt) in enumerate(pts):
                emit_E(pi, tt, 1)
                if k + 1 < len(pts):
                    pn, tn = pts[k + 1]
                    if tn == 0:
                        pair_loads(pn)
                    emit_E(pn, tn, 0)
                emit_D(pi, tt)
            P.barrier()

        out_groups = []
        x_issued = set()

        def issue_x(st_, blk):
            si = blk % 4
            g = P.new_group("xs%d" % si)
            P.add("sync", lambda e, si=si, blk=blk, st_=st_: e.dma_start(out=xs[si], in_=x[st_ * TS + blk * 128:st_ * TS + (blk + 1) * 128, :]),
                  writes=[("xs", si)], grp=g)
            x_issued.add((st_, blk))
        def do_super(st):
            t0 = st * TS
            for blk in range(NBLK):
                si = blk % 4
                if (st, blk) not in x_issued:
                    issue_x(st, blk)
                tt = blk // 4
                for half in range(2):
                    b = nextbank()
                    for k4 in range(4):
                        kc = half * 4 + k4
                        P.add("pe", lambda e, si=si, kc=kc, k4=k4, b=b: e.transpose(bank(b)[:, k4 * 128:(k4 + 1) * 128], xs[si][:, kc:1024:8], ident),
                              reads=[("xs", si), "ident"], writes=[("ps", b)])
                    eng = "dve" if half == 0 else "act"
                    dst = h[:, half * 4:(half + 1) * 4, blk * 128:(blk + 1) * 128]
                    src = bank(b).rearrange("p (a b) -> p a b", a=4)
                    if eng == "dve":
                        P.add("dve", lambda e, dst=dst, src=src: e.tensor_copy(dst, src), reads=[("ps", b)], writes=[HK(half * 4 + k, tt) for k in range(4)])
                    else:
                        P.add("act", lambda e, dst=dst, src=src: e.activation(out=dst, in_=src, func=AF.Copy), reads=[("ps", b)], writes=[HK(half * 4 + k, tt) for k in range(4)])
                if blk % 4 == 3:
                    norm_stats(blk // 4)
            P.barrier()

            if stop_stage >= 1:
                state["off"] = arena_base
                ym = alloc([128, 8, TS], BF16)
                rmsnorm(0, ym)
                qT = alloc([128, 4, TS], BF16); kTw = alloc([128, 4, 512 + TS], BF16); Vw = alloc([128, 4 + NBLK, 8, 128], BF16)
                pTg = [alloc([128, 16 + TS]) for _ in range(2)]; tA = alloc([128, 16 + TS]); tB = alloc([128, 16 + TS])
                pooled = alloc([128, TS], BF16); mixP = alloc([128, 4, TS], BF16)
                Pb = [alloc([128, 512], BF16) for _ in range(4)]; Rb = [alloc([128, 128]) for _ in range(2)]
                MK = lambda kc, tt: ("mix", kc, tt)
                if st > 0:
                    P.add("dve", lambda e: e.tensor_copy(kTw[:, :, 0:512], ktail), reads=["ktail"], writes=[("kT", j) for j in range(4)])
                    P.add("dve", lambda e: e.tensor_copy(Vw[:, 0:4].rearrange("p a b c -> p (a b c)"), vtail.rearrange("p a b c -> p (a b c)")),
                          reads=["vtail"], writes=[("V", b_) for b_ in range(4)])
                for b_ in range(NBLK):
                    P.add("dve", lambda e, b_=b_: e.memset(Vw[:, 4 + b_, :, 64:128], 1.0), writes=[("V", 4 + b_)])
                wsl = load_w_in(ab_w_in)
                def proj_qk(which, slot_i):
                    for j in range(4):
                        for tt in range(NT):
                            ts_ = slice(tt * 512, (tt + 1) * 512)
                            b = nextbank()
                            w = ring[wsl[slot_i]].rearrange("p (a b) -> p a b", a=8)
                            for kc in range(8):
                                P.add("pe", lambda e, w=w, kc=kc, j=j, b=b, ts_=ts_: e.matmul(bank(b), lhsT=w[:, kc, j * 128:(j + 1) * 128], rhs=ym[:, kc, ts_],
                                                                                         start=(kc == 0), stop=(kc == 7)),
                                      reads=[("ring", wsl[slot_i]), YK(kc, tt)], writes=[("ps", b)])
                            if which == "q":
                                P.add("act", lambda e, j=j, b=b, ts_=ts_: e.activation(out=qT[:, j, ts_], in_=bank(b), func=AF.Copy),
                                      reads=[("ps", b)], writes=[("qT", j)])
                            else:
                                P.add("dve", lambda e, j=j, b=b, tt=tt: e.tensor_copy(kTw[:, j, 512 + tt * 512:512 + (tt + 1) * 512], bank(b)),
                                      reads=[("ps", b)], writes=[("kT", j)])
                def proj_v(blks):
                    wv = ring[wsl[3]].rearrange("p (a b) -> p a b", a=8)
                    for blk in blks:
                        b = nextbank(); tt = blk // 4
                        for kc in range(8):
                            P.add("pe", lambda e, kc=kc, blk=blk, b=b: e.matmul(bank(b), lhsT=ym[:, kc, blk * 128:(blk + 1) * 128], rhs=wv[:, kc, :],
                                                                          start=(kc == 0), stop=(kc == 7)),
                                  reads=[("ring", wsl[3]), YK(kc, tt)], writes=[("ps", b)])
                        eng = "act" if blk % 2 else "dve"
                        dst = Vw[:, 4 + blk, :, 0:64]; src = bank(b).rearrange("p (a b) -> p a b", a=8)
                        if eng == "dve":
                            P.add("dve", lambda e, dst=dst, src=src: e.tensor_copy(dst, src), reads=[("ps", b)], writes=[("V", 4 + blk)])
                        else:
                            P.add("act", lambda e, dst=dst, src=src: e.activation(out=dst, in_=src, func=AF.Copy), reads=[("ps", b)], writes=[("V", 4 + blk)])
                wp = ring[wsl[0]].rearrange("p (a b) -> p a b", a=8)
                def pool_proj(g_):
                    pt = pTg[g_ % 2]; PK = ("pT", g_ % 2)
                    if st > 0:
                        P.add("dve", lambda e, pt=pt, g_=g_: e.tensor_copy(pt[:, 0:16], ptail[:, g_, :]), reads=["ptail"], writes=[PK])
                    else:
                        P.add("dve", lambda e, pt=pt: e.memset(pt[:, 0:16], 0.0), writes=[PK])
                    for tt in range(NT):
                        ts_ = slice(tt * 512, (tt + 1) * 512)
                        b = nextbank()
                        for kc in range(8):
                            P.add("pe", lambda e, kc=kc, g_=g_, b=b, ts_=ts_: e.matmul(bank(b), lhsT=wp[:, kc, g_ * 128:(g_ + 1) * 128], rhs=ym[:, kc, ts_],
                                                                                 start=(kc == 0), stop=(kc == 7)),
                                  reads=[("ring", wsl[0]), YK(kc, tt)], writes=[("ps", b)])
                        P.add("act", lambda e, pt=pt, b=b, tt=tt: e.activation(out=pt[:, 16 + tt * 512:16 + (tt + 1) * 512], in_=bank(b), func=AF.Copy),
                              reads=[("ps", b)], writes=[PK])
                    P.add("dve", lambda e, pt=pt, g_=g_: e.tensor_copy(ptail[:, g_, :], pt[:, TS:TS + 16]), reads=[PK], writes=["ptail"])
                def pool_chain(g_):
                    pt = pTg[g_ % 2]; PK = ("pT", g_ % 2)
                    W_ = 16 + TS
                    srcb = pt; bufs = [tA, tB]; keys = ["tA", "tB"]; sk = PK
                    for lev in range(g_ + 1):
                        sh = 1 << lev; lo = (1 << (lev + 1)) - 1
                        dstb = bufs[lev % 2]; dk = keys[lev % 2]
                        P.add("dve", lambda e, dstb=dstb, srcb=srcb, sh=sh, lo=lo: e.tensor_tensor(out=dstb[:, lo:W_], in0=srcb[:, lo:W_], in1=srcb[:, lo - sh:W_ - sh], op=ALU.add),
                              reads=[sk], writes=[dk])
                        srcb = dstb; sk = dk
                    wdw = float(1 << (g_ + 1))
                    P.add("dve", lambda e, srcb=srcb, pt=pt, wdw=wdw: e.scalar_tensor_tensor(out=pooled, in0=srcb[:, 16:W_], scalar=1.0 / wdw, in1=pt[:, 16:W_],
                                                                                       op0=ALU.mult, op1=ALU.subtract), reads=[sk, PK], writes=["pooled"])
                    if st == 0:
                        P.add("dve", lambda e, srcb=srcb, g_=g_: e.tensor_tensor(out=srcb[:, 0:16], in0=srcb[:, 16:32], in1=invcnt[:, g_, :], op=ALU.mult),
                              reads=[sk, "invcnt"], writes=[sk])
                        P.add("dve", lambda e, srcb=srcb, pt=pt: e.tensor_tensor(out=pooled[:, 0:16], in0=srcb[:, 0:16], in1=pt[:, 16:32], op=ALU.subtract),
                              reads=[sk, PK, "pooled"], writes=["pooled"])
                def pool_mm(g_):
                    for tt in range(NT):
                        ts_ = slice(tt * 512, (tt + 1) * 512)
                        b = nextbank()
                        P.add("pe", lambda e, g_=g_, b=b, ts_=ts_: e.matmul(bank(b), lhsT=poolw[:, g_, :], rhs=pooled[:, ts_], start=True, stop=True),
                              reads=["poolw", "pooled"], writes=[("ps", b)])
                        P.add("dve", lambda e, g_=g_, b=b, ts_=ts_: e.tensor_scalar(out=mixP[:, g_, ts_], in0=bank(b), scalar1=pscale[:, g_:g_ + 1], scalar2=None, op0=ALU.mult),
                              reads=[("ps", b), "pscale"], writes=[MK(g_, tt)])
                pool_proj(0); pool_proj(1); proj_qk("q", 1); pool_chain(0); pool_mm(0); pool_proj(2); proj_qk("k", 2)
                pool_chain(1); pool_mm(1); pool_proj(3); pool_chain(2); proj_v(range(0, NBLK // 2)); pool_mm(2)
                pool_chain(3); proj_v(range(NBLK // 2, NBLK)); pool_mm(3)
                wo = load_w_out(ab_w_out)
                lbmin = 4 if st == 0 else 0
                LA = 3
                osb = [(tA[:, 0:512], "tA"), (tB[:, 0:512], "tB"), (pTg[0][:, 0:512], ("pT", 0)), (pTg[1][:, 0:512], ("pT", 1))]
                Rn = pooled.bitcast(F32)
                items = []
                for hh in range(8):
                    for half in range(2):
                        mr0 = half * 4; mr1 = half * 4 + 3
                        first = True
                        for lb in range(max(lbmin, mr0), mr1 + 5):
                            m_lo = max(lb - 4, mr0); m_hi = min(lb, mr1)
                            if m_lo <= m_hi:
                                items.append((hh, half, lb, m_lo, m_hi, first)); first = False

                def emit_qk(i):
                    hh, half, lb, m_lo, m_hi, first = items[i]
                    j = hh // 2; po = (hh % 2) * 64
                    nq = (m_hi - m_lo + 1) * 128; q0 = m_lo * 128
                    sb = i % 4
                    P.add("pe", lambda e, j=j, po=po, lb=lb, nq=nq, q0=q0, sb=sb: e.matmul(
                        bank(sb)[:, 0:nq], lhsT=kTw[po:po + 64, j, lb * 128:(lb + 1) * 128], rhs=qT[po:po + 64, j, q0:q0 + nq], start=True, stop=True),
                        reads=[("kT", j), ("qT", j)], writes=[("ps", sb)])

                def emit_rest(i, deferred):
                    hh, half, lb, m_lo, m_hi, first = items[i]
                    mr1_ = half * 4 + 3
                    j = hh // 2; po = (hh % 2) * 64
                    ob = 4 + (hh * 2 + half) % 4
                    OT = bank(ob)
                    if first:
                        P.add("dve", lambda e, OT=OT: e.memset(OT, 0.0), writes=[("ps", ob)])
                    nq = (m_hi - m_lo + 1) * 128
                    sb = i % 4; pi_ = i % 4
                    P.add("act", lambda e, pi_=pi_, sb=sb, nq=nq: e.activation(out=Pb[pi_][:, 0:nq], in_=bank(sb)[:, 0:nq], func=AF.Exp, scale=ATT_SCALE),
                          reads=[("ps", sb)], writes=[("Pb", pi_)])
                    has0 = (m_lo == lb - 4); has1 = (m_lo <= lb - 3 <= m_hi)
                    if has0 and has1:
                        pc, ec = 0, (0, 256)
                    elif has0:
                        pc, ec = 0, (0, 128)
                    elif has1:
                        pc, ec = (lb - 3 - m_lo) * 128, (128, 256)
                    else:
                        pc = None
                    if pc is not None:
                        P.add("dve", lambda e, pi_=pi_, pc=pc, ec=ec, hh=hh: e.tensor_tensor(out=Pb[pi_][:, pc:pc + ec[1] - ec[0]], in0=Pb[pi_][:, pc:pc + ec[1] - ec[0]],
                                                                                       in1=etab[:, hh, ec[0]:ec[1]], op=ALU.mult),
                              reads=[("Pb", pi_), "etab"], writes=[("Pb", pi_)])
                    while deferred and deferred[0][0] <= i - 2:
                        deferred.pop(0)[1]()
                    vfull = Vw[:, lb, hh, :]
                    vhalf = Vw[64:128, lb, hh, :]
                    m = m_lo
                    while m <= m_hi:
                        last = (lb == m + 4); nat = (lb == m)
                        pc0 = (m - m_lo) * 128; oc = (m - half * 4) * 128
                        if nat:
                            P.add("pe", lambda e, oc=oc, pc0=pc0, pi_=pi_, OT=OT, vfull=vfull, last=last: e.matmul(
                                OT[:, oc:oc + 64], lhsT=vfull, rhs=Pb[pi_][:, pc0:pc0 + 64], start=False, stop=last, skip_group_check=True),
                                reads=[("V", lb), ("Pb", pi_)], writes=[("ps", ob)])
                            P.add("pe", lambda e, oc=oc, pc0=pc0, pi_=pi_, OT=OT, vhalf=vhalf, last=last: e.matmul(
                                OT[:, oc + 64:oc + 128], lhsT=vhalf, rhs=Pb[pi_][64:128, pc0 + 64:pc0 + 128], start=False, stop=last, skip_group_check=True),
                                reads=[("V", lb), ("Pb", pi_)], writes=[("ps", ob)])
                            m2_ = m + 1
                        else:
                            m2_ = m + 1
                            while (m2_ <= m_hi and (lb == m2_ + 4) == last and lb != m2_):
                                m2_ += 1
                            nn = (m2_ - m) * 128
                            P.add("pe", lambda e, oc=oc, nn=nn, pc0=pc0, pi_=pi_, OT=OT, vfull=vfull, last=last: e.matmul(
                                OT[:, oc:oc + nn], lhsT=vfull, rhs=Pb[pi_][:, pc0:pc0 + nn], start=False, stop=last, skip_group_check=True),
                                reads=[("V", lb), ("Pb", pi_)], writes=[("ps", ob)])
                        if lb == mr1_ + 4 and m2_ > mr1_:
                            u_ = hh * 2 + half
                            OS, osk = osb[u_ % 4]

                            def norm_(OS=OS, osk=osk, OT=OT, j=j, po=po, ob=ob, half=half):
                                P.add("dve", lambda e: e.tensor_copy(OS, OT), reads=[("ps", ob)], writes=[osk])
                                P.add("act", lambda e: e.activation(out=Rn[0:64, :], in_=OS[64:128, :], func=AF.Ln), reads=[osk], writes=["pooled"])
                                P.add("act", lambda e: e.activation(out=Rn[0:64, :], in_=Rn[0:64, :], func=AF.Exp, scale=-1.0), reads=["pooled"], writes=["pooled"])
                                P.add("dve", lambda e: e.tensor_tensor(out=ym[po:po + 64, j, half * 512:(half + 1) * 512], in0=OS[0:64, :], in1=Rn[0:64, :], op=ALU.mult),
                                      reads=[osk, "pooled"], writes=[YK(j, half)])
                            deferred.append((i, norm_))
                        m = m2_

                for i in range(min(LA, len(items))):
                    emit_qk(i)
                deferred = []
                for i in range(len(items)):
                    if i + LA < len(items):
                        emit_qk(i + LA)
                    emit_rest(i, deferred)
                for _, fn_ in deferred:
                    fn_()
                P.add("dve", lambda e: e.tensor_copy(ktail, kTw[:, :, TS:TS + 512]), reads=[("kT", j) for j in range(4)], writes=["ktail"])
                P.add("dve", lambda e: e.tensor_copy(vtail.rearrange("p a b c -> p (a b c)"), Vw[:, NBLK:NBLK + 4].rearrange("p a b c -> p (a b c)")),
                      reads=[("V", b_) for b_ in range(NBLK, NBLK + 4)], writes=["vtail"])
                for tt in range(NT):
                    ts_ = slice(tt * 512, (tt + 1) * 512)
                    for kco in range(8):
                        b = nextbank()
                        for kc in range(8):
                            w = ring[wo[kc // 4]].rearrange("p (a b) -> p a b", a=4)
                            srcm = mixP[:, kc, ts_] if kc < 4 else ym[:, kc - 4, ts_]
                            P.add("pe", lambda e, w=w, kc=kc, kco=kco, b=b, srcm=srcm: e.matmul(bank(b), lhsT=w[:, kc % 4, kco:1024:8], rhs=srcm,
                                                                                         start=(kc == 0), stop=(kc == 7)),
                                  reads=[("ring", wo[kc // 4]), MK(kc, tt) if kc < 4 else YK(kc - 4, tt)], writes=[("ps", b)])
                        P.add("dve", lambda e, kco=kco, b=b, ts_=ts_: e.tensor_tensor(out=h[:, kco, ts_], in0=bank(b), in1=h[:, kco, ts_], op=ALU.add),
                              reads=[("ps", b), HK(kco, tt)], writes=[HK(kco, tt)])
                    norm_stats(tt)
                P.barrier()
            if stop_stage >= 2:
                moe_phase(0)
            if stop_stage >= 3:
                state["off"] = arena_base
                ym = alloc([128, 8, TS], BF16)
                bvB, bsB, wsT32, trim, lnfm = sgu_const_views()
                Cb = alloc([128, 8, 128]); wsT = alloc([128, 8, 128], BF16)
                P.add("dve", lambda e: e.tensor_tensor(out=wsT, in0=wsT32, in1=trim.unsqueeze(1).broadcast_to([128, 8, 128]), op=ALU.mult),
                      reads=["wsT32", "trim"], writes=["wsT"])
                for half in range(2):
                    b = nextbank()
                    P.add("pe", lambda e, half=half, b=b: e.matmul(bank(b), lhsT=ones_bf, rhs=wsT.rearrange("p a b -> p (a b)")[:, half * 512:(half + 1) * 512], start=True, stop=True),
                          reads=["wsT", "ones"], writes=[("ps", b)])
                    for h4 in range(4):
                        hh = half * 4 + h4
                        P.add("dve", lambda e, hh=hh, h4=h4, b=b: e.scalar_tensor_tensor(out=Cb[:, hh, :], in0=bank(b)[:, h4 * 128:(h4 + 1) * 128], scalar=lnfm[:, 1, hh:hh + 1],
                                                                                   in1=bsB[:, hh, :], op0=ALU.mult, op1=ALU.add),
                              reads=[("ps", b), "lnfm", "bsB"], writes=["Cb"])
                rmsnorm(2, ym)
                uT = alloc([128, 8, TS], BF16); vn = alloc([128, NBLK, 1024], BF16)
                vpre = [alloc([128, 1024]) for _ in range(2)]; tmp5 = [alloc([128, 512]) for _ in range(2)]
                stats = alloc([128, NBLK, 2, 6]); mv = alloc([128, NBLK, 2]); lrs = alloc([128, NBLK]); nmean = alloc([128, NBLK])
                assert state["off"] <= arena_base + SGU_C_OFF, state["off"] - arena_base
                wsl = load_w_in(sgu_w_in, order=(2, 3, 0, 1))
                for blk in range(NBLK):
                    tt = blk // 4; vi = blk % 2
                    for half in range(2):
                        w = ring[wsl[2 + half]].rearrange("p (a b) -> p a b", a=8)
                        b = nextbank()
                        for kc in range(8):
                            P.add("pe", lambda e, w=w, kc=kc, blk=blk, b=b: e.matmul(bank(b), lhsT=ym[:, kc, blk * 128:(blk + 1) * 128], rhs=w[:, kc, :],
                                                                               start=(kc == 0), stop=(kc == 7)),
                                  reads=[("ring", wsl[2 + half]), YK(kc, tt)], writes=[("ps", b)])
                        P.add("dve", lambda e, vi=vi, half=half, b=b: e.tensor_tensor(out=vpre[vi][:, half * 512:(half + 1) * 512], in0=bank(b),
                                                                                  in1=bvB[:, half * 512:(half + 1) * 512], op=ALU.add),
                              reads=[("ps", b), "bvB"], writes=[("vpre", vi)])
                    P.add("act", lambda e, vi=vi, blk=blk: e.activation(out=vn[:, blk, :], in_=vpre[vi], func=AF.Gelu), reads=[("vpre", vi)], writes=[("vn", blk)])
                    for half in range(2):
                        P.add("dve", lambda e, half=half, blk=blk: e.bn_stats(out=stats[:, blk, half, :], in_=vn[:, blk, half * 512:(half + 1) * 512]),
                              reads=[("vn", blk)], writes=["stats"])
                    P.add("dve", lambda e, blk=blk: e.bn_aggr(out=mv[:, blk, :], in_=stats[:, blk, :, :]), reads=["stats"], writes=["mv"])
                for c in range(8):
                    w = ring[wsl[c // 4]].rearrange("p (a b) -> p a b", a=8)
                    for tt in range(NT):
                        ts_ = slice(tt * 512, (tt + 1) * 512)
                        b = nextbank()
                        for kc in range(8):
                            P.add("pe", lambda e, w=w, kc=kc, c=c, b=b, ts_=ts_: e.matmul(bank(b), lhsT=w[:, kc, (c % 4) * 128:(c % 4 + 1) * 128], rhs=ym[:, kc, ts_],
                                                                                     start=(kc == 0), stop=(kc == 7)),
                                  reads=[("ring", wsl[c // 4]), YK(kc, tt)], writes=[("ps", b)])
                        P.add("act", lambda e, c=c, b=b, ts_=ts_: e.activation(out=uT[:, c, ts_], in_=bank(b), func=AF.Gelu, bias=b_u[:, c:c + 1]),
                              reads=[("ps", b), "b_u"], writes=[("uT", c, tt)])
                P.add("act", lambda e: e.activation(out=lrs, in_=mv[:, :, 1], func=AF.Ln, bias=epst[:, 0:1]), reads=["mv", "eps"], writes=["lrs"])
                P.add("act", lambda e: e.activation(out=lrs, in_=lrs, func=AF.Exp, scale=-0.5), reads=["lrs"], writes=["lrs"])
                wo = load_w_out(sgu_w_out)
                P.add("dve", lambda e: e.scalar_tensor_tensor(out=nmean, in0=mv[:, :, 0], scalar=-1.0, in1=lrs, op0=ALU.mult, op1=ALU.mult),
                      reads=["mv", "lrs"], writes=["nmean"])
                for blk in range(NBLK):
                    P.add("act", lambda e, blk=blk: e.activation(out=vn[:, blk, :], in_=vn[:, blk, :], func=AF.Identity, scale=lrs[:, blk:blk + 1], bias=nmean[:, blk:blk + 1]),
                          reads=[("vn", blk), "nmean", "lrs"], writes=[("vn", blk)])
                for half in range(NBLK // 4):
                    for hh in range(8):
                        b = nextbank(); ti = hh % 2
                        for b4 in range(4):
                            blk = half * 4 + b4
                            P.add("pe", lambda e, hh=hh, blk=blk, b4=b4, b=b: e.matmul(bank(b)[:, b4 * 128:(b4 + 1) * 128], lhsT=vn[:, blk, hh * 128:(hh + 1) * 128],
                                                                                  rhs=wsT[:, hh, :], start=True, stop=True),
                                  reads=[("vn", blk), "wsT"], writes=[("ps", b)])
                        P.add("dve", lambda e, hh=hh, b=b, ti=ti: e.scalar_tensor_tensor(out=tmp5[ti].rearrange("p (a b) -> p a b", a=4), in0=bank(b).rearrange("p (a b) -> p a b", a=4),
                                                                                    scalar=lnfm[:, 0, hh:hh + 1], in1=Cb[:, hh, :].unsqueeze(1).broadcast_to([128, 4, 128]),
                                                                                    op0=ALU.mult, op1=ALU.add),
                              reads=[("ps", b), "Cb", "lnfm"], writes=[("tmp5", ti)])
                        P.add("dve", lambda e, hh=hh, half=half, ti=ti: e.tensor_tensor(out=ym[:, hh, half * 512:(half + 1) * 512], in0=tmp5[ti],
                                                                                   in1=uT[:, hh, half * 512:(half + 1) * 512], op=ALU.mult),
                              reads=[("tmp5", ti), ("uT", hh, half)], writes=[YK(hh, half)])
                proj_to_h(wo, ym)
                P.barrier()
            if stop_stage >= 4:
                moe_phase(1)
            state["off"] = arena_base
            yf = alloc([128, 8, TS])
            if st + 1 < n_super:
                issue_x(st + 1, 0); issue_x(st + 1, 1); issue_x(st + 1, 2); issue_x(st + 1, 3)
            if stop_stage >= 5:
                rmsnorm(4, yf)
                src_t = yf; SK = YK
            else:
                src_t = h; SK = HK
            osl = [alloc([128, D]) for _ in range(4)]
            assert state["off"] <= arena_base + XS_OFF, state["off"] - arena_base
            for blk in range(NBLK):
                si = blk % 4; tt = blk // 4
                ov = osl[si].rearrange("t (p kc) -> t kc p", kc=8)
                for half in range(2):
                    b = nextbank()
                    for k4 in range(4):
                        kc = half * 4 + k4
                        P.add("pe", lambda e, kc=kc, k4=k4, b=b, blk=blk: e.transpose(bank(b)[:, k4 * 128:(k4 + 1) * 128], src_t[:, kc, blk * 128:(blk + 1) * 128], ident),
                              reads=[SK(kc, tt), "ident"], writes=[("ps", b)])
                    dst = ov[:, half * 4:(half + 1) * 4, :]; src = bank(b).rearrange("p (a b) -> p a b", a=4)
                    P.add("act", lambda e, dst=dst, src=src: e.activation(out=dst, in_=src, func=AF.Copy), reads=[("ps", b)], writes=[("os", si)])
                g = P.new_group("os%d" % si)
                P.add("sync", lambda e, si=si, blk=blk, t0=t0: e.dma_start(out=out[t0 + blk * 128:t0 + (blk + 1) * 128, :], in_=osl[si]),
                      reads=[("os", si)], writes=[], grp=g)
                out_groups.append(g)
            if st == n_super - 1:
                P.barrier()
        for st_ in range(n_super):
            do_super(st_)
        P.final_tokens = out_groups[-4:]
        P.emit(nc, es)
    return nc


def _host_consts(inp):
    c = {}
    f = lambda a: np.ascontiguousarray(a, dtype=np.float32)
    gains = np.stack([inp["norm_mix_g"][0], inp["norm_ffn_g"][0], inp["norm_mix_g"][1], inp["norm_ffn_g"][1], inp["final_norm_g"]], 0)
    c["gains"] = f(gains.reshape(5, 128, 8).transpose(1, 0, 2).reshape(128, 40))
    c["ab_w_in"] = f(inp["ab_w_in"][0]); c["pool_w"] = f(inp["pool_w"][0])
    c["pscale"] = f(inp["pool_scale"][0].reshape(4, 128).T)
    rb = inp["att_rel_bias"][0]
    p = np.arange(128)[:, None, None]; i = np.arange(64)[None, None, :]
    order = [0, 1, 2, 3]
    idx = np.concatenate([np.clip(p - 64 * d - i, -128, 128) + 128 for d in order], axis=2)
    idx = np.broadcast_to(idx, (128, 8, 256))
    hh = np.arange(8)[None, :, None]
    c["btab"] = f(rb[hh, idx].reshape(128, 2048))
    c["r0tab"] = f(np.broadcast_to(rb[:, 0][None, :, None], (128, 8, 256)).reshape(128, 2048))
    em = np.ones((128, 256), np.float32); em[64:, 0:64] = 0.0
    c["emask"] = em
    c["ab_w_out"] = f(inp["ab_w_out"][0]); c["sgu_w_in"] = f(inp["sgu_w_in"][0])
    c["bu"] = f(inp["sgu_b_in"][0][:1024].reshape(8, 128).T); c["bv"] = f(inp["sgu_b_in"][0][1024:])
    c["lnfm"] = f(np.stack([inp["sgu_ln_g"][0].reshape(8, 128).T, inp["sgu_ln_b"][0].reshape(8, 128).T], axis=1).reshape(128, 16))
    c["wsT"] = f(inp["sgu_w_s"][0].transpose(2, 0, 1).reshape(128, 1024))
    c["trimask"] = f(np.triu(np.ones((128, 128), np.float32)))
    c["bs"] = f(inp["sgu_b_s"][0].reshape(1024)); c["sgu_w_out"] = f(inp["sgu_w_out"][0])
    wr = [np.concatenate([inp["moe_wg_router"][l], inp["moe_we_router"][l].transpose(1, 0, 2).reshape(D, 16)], axis=1) for l in range(2)]
    c["wr"] = f(np.stack(wr, 0))
    c["rbias"] = f(np.concatenate([np.concatenate([inp["moe_bg_router"][l], inp["moe_be_router"][l].reshape(16)]) for l in range(2)]))
    c["w_gate"] = f(inp["moe_w_gate"]); c["w_up"] = f(inp["moe_w_up"]); c["w_down"] = f(inp["moe_w_down"])
    c["ident"] = np.eye(128, dtype=np.float32)
    se = np.zeros((32, 16, 128), np.float32)
    for e in range(16):
        se[e, e, :] = 1.0; se[16 + e, e, :] = 1.0
    c["sele"] = se.reshape(32, 2048)
    ic = np.zeros((128, 4, 16), np.float32)
    for g in range(4):
        w = 2 ** (g + 1)
        ic[:, g, :] = 1.0 / np.minimum(np.arange(16) + 1, w)
    c["invcnt"] = ic.reshape(128, 64)
    return c


_NC_CACHE = {}


def kernel(**inputs):
    inp = {k: np.asarray(v) for k, v in inputs.items()}
    consts = _host_consts(inp)
    if "nc" not in _NC_CACHE:
        _NC_CACHE["nc"] = build_program()
    nc = _NC_CACHE["nc"]
    xin = np.ascontiguousarray(inp["x"], dtype=np.float32)
    in_maps = []
    for b in range(8):
        m = dict(consts); m["x"] = xin[b]
        in_maps.append(m)
    res = run_bass_kernel_spmd(nc, in_maps, core_ids=list(range(8)))
    return np.stack([np.asarray(r["out"], dtype=np.float32) for r in res.results], axis=0)
```

```python
import numpy as np
from contextlib import ExitStack
import concourse.bass as bass
import concourse.mybir as mybir
from concourse.bass_utils import run_bass_kernel_spmd

F32 = mybir.dt.float32
BF16 = mybir.dt.bfloat16
AF = mybir.ActivationFunctionType
ALU = mybir.AluOpType
AX = mybir.AxisListType

D = 1024
SEQ = 4096
TS = 1024
NT = TS // 512
NBLK = TS // 128
EPS = 1e-6
ATT_SCALE = 64 ** -0.5
ENGS = ["sync", "act", "pool", "dve", "pe"]


class Op:
    __slots__ = ("eng", "fn", "deps", "sig", "sigidx", "grp", "is_dma")

    def __init__(self, eng, fn, is_dma=False, grp=None):
        self.eng = eng; self.fn = fn; self.deps = set(); self.sig = False
        self.sigidx = None; self.grp = grp; self.is_dma = is_dma


class Grp:
    def __init__(self, sem):
        self.sem = sem; self.val = 0


class Prog:
    def __init__(self):
        self.ops = {e: [] for e in ENGS}
        self.tiles = {}
        self.dma_cnt = {}
        self.pending = {e: set() for e in ENGS}
        self.final_tokens = []

    def new_group(self, sem):
        g = Grp(sem)
        g.val = self.dma_cnt.get(sem, 0)
        return g

    def add(self, eng, fn, reads=(), writes=(), grp=None):
        op = Op(eng, fn, is_dma=grp is not None, grp=grp)
        if grp is not None:
            self.dma_cnt[grp.sem] = self.dma_cnt.get(grp.sem, 0) + 16
            grp.val = self.dma_cnt[grp.sem]
        raw = set(); oth = set()
        for k in reads:
            st = self.tiles.get(k)
            if st is not None and st[0] is not None:
                raw.add(st[0])
        for k in writes:
            st = self.tiles.get(k)
            if st is not None:
                if st[0] is not None:
                    oth.add(st[0])
                oth.update(st[1].values())
        for d in raw | oth:
            if d is op:
                continue
            if d.grp is not None and d.grp is op.grp:
                continue
            if d.is_dma or op.is_dma or d.eng != eng:
                op.deps.add(d)
            elif eng != "pe":
                op.deps.add(d)
        for d in self.pending[eng]:
            op.deps.add(d)
        self.pending[eng] = set()
        for k in reads:
            self.tiles.setdefault(k, [None, {}])[1][eng if not op.is_dma else id(op)] = op
        for k in writes:
            self.tiles[k] = [op, {}]
        self.ops[eng].append(op)
        return op

    def barrier(self, engs=("sync", "act", "dve", "pe")):
        lasts = {e: self.ops[e][-1] for e in engs if self.ops[e]}
        n0 = getattr(self, "_sync_mark", 0)
        dmas = [o for o in self.ops["sync"][n0:] if o.is_dma]
        self._sync_mark = len(self.ops["sync"])
        for e in engs:
            for e2, o in lasts.items():
                if e2 != e:
                    self.pending[e].add(o)
            for o in dmas:
                self.pending[e].add(o)

    def emit(self, nc, es):
        for e in ENGS:
            for op in self.ops[e]:
                for d in op.deps:
                    if not d.is_dma:
                        d.sig = True
        for e in ENGS:
            n = 0
            for op in self.ops[e]:
                if op.sig and not op.is_dma:
                    n += 1; op.sigidx = n
        sems = {e: es.enter_context(nc.semaphore("s_" + e)) for e in ENGS}
        dsems = {}
        for e in ENGS:
            for op in self.ops[e]:
                if op.is_dma and op.grp.sem not in dsems:
                    dsems[op.grp.sem] = es.enter_context(nc.semaphore("d_" + op.grp.sem))
        block = es.enter_context(nc.Block())
        final_tokens = self.final_tokens

        def run(e, eng):
            waited = {}
            for op in self.ops[e]:
                need = {}
                for d in op.deps:
                    if d.is_dma:
                        s, v = dsems[d.grp.sem], d.grp.val
                    else:
                        s, v = sems[d.eng], d.sigidx
                    key = id(s)
                    if waited.get(key, 0) < v:
                        if key not in need or need[key][1] < v:
                            need[key] = (s, v)
                for key, (s, v) in need.items():
                    eng.wait_ge(s, v); waited[key] = v
                inst = op.fn(eng)
                if op.is_dma:
                    inst.then_inc(dsems[op.grp.sem], 16)
                elif op.sig:
                    inst.then_inc(sems[e], 1)
            if e == "sync":
                for g in final_tokens:
                    eng.wait_ge(dsems[g.sem], g.val)

        block.sync(lambda eng: run("sync", eng))
        block.scalar(lambda eng: run("act", eng))
        block.gpsimd(lambda eng: run("pool", eng))
        block.vector(lambda eng: run("dve", eng))
        block.tensor(lambda eng: run("pe", eng))


FAST_RECIP = False


def RECIP(e, out, in_):
    if FAST_RECIP:
        return e.reciprocal_approx_fast(out=out, in_=in_)
    return e.reciprocal(out=out, in_=in_)


def build_program(n_super=SEQ // TS, stop_stage=99):
    nc = bass.Bass("TRN2", target_bir_lowering=False)
    P = Prog()

    def din(name, shape):
        return nc.dram_tensor(name, list(shape), F32, kind="ExternalInput").ap()

    x = din("x", [SEQ, D])
    out = nc.dram_tensor("out", [SEQ, D], F32, kind="ExternalOutput").ap()
    gains_d = din("gains", [128, 5 * 8])
    ab_w_in = din("ab_w_in", [D, 2048]); pool_w = din("pool_w", [4, 128, 128])
    pscale_d = din("pscale", [128, 4]); btab_d = din("btab", [128, 8 * 256]); r0tab_d = din("r0tab", [128, 8 * 256])
    emask_d = din("emask", [128, 256]); ab_w_out = din("ab_w_out", [D, D])
    sgu_w_in = din("sgu_w_in", [D, 2048]); bu_d = din("bu", [128, 8]); bv_d = din("bv", [1024])
    lnfm_d = din("lnfm", [128, 16]); wsT_d = din("wsT", [128, 1024])
    trimask_d = din("trimask", [128, 128]); bs_d = din("bs", [1024]); sgu_w_out = din("sgu_w_out", [D, D])
    wr_d = din("wr", [2, D, 20]); rb_d = din("rbias", [40])
    w_gate = din("w_gate", [2, 16, D, 256]); w_up = din("w_up", [2, 16, D, 256]); w_down = din("w_down", [2, 16, 256, D])
    ident_d = din("ident", [128, 128]); sele_d = din("sele", [32, 16 * 128]); invcnt_d = din("invcnt", [128, 64])

    es = ExitStack()
    with es:
        avail = (nc.sbuf_bytes_remaining // 256) * 256 - 256
        arena = es.enter_context(nc.sbuf_tensor("arena", [128, avail // 4], F32))
        psum = es.enter_context(nc.psum_tensor("psum", [128, 4096], F32))
        state = {"off": 0}

        def alloc(shape, dtype=F32, parts=None):
            n = int(np.prod(shape[1:]))
            nbytes = n * (4 if dtype == F32 else 2)
            nbytes = (nbytes + 63) // 64 * 64
            o = state["off"]; state["off"] += nbytes
            assert state["off"] <= avail, ("SBUF overflow", state["off"], avail)
            v = arena[:, o // 4:(o + nbytes) // 4]
            if dtype != F32:
                v = v.bitcast(dtype)
            v = v[:, 0:n]
            if len(shape) == 3:
                v = v.rearrange("p (a b) -> p a b", a=shape[1])
            elif len(shape) == 4:
                v = v.rearrange("p (a b c) -> p a b c", a=shape[1], b=shape[2])
            return v

        def bank(b, n=1):
            return psum[:, b * 512:(b + n) * 512]

        rr = {"b": 0}

        def nextbank():
            b = rr["b"]; rr["b"] = (b + 1) % 8
            return b

        h = alloc([128, 8, TS])
        NSLOT = 6
        ring = [alloc([128, 4096], BF16) for _ in range(NSLOT)]
        ktail = alloc([128, 4, 512], BF16); vtail = alloc([128, 4, 8, 128], BF16); ptail = alloc([128, 4, 16])
        etab = alloc([128, 8, 256], BF16)
        ident = alloc([128, 128]); ones_bf = alloc([128, 128], BF16); gains = alloc([128, 5, 8])
        pscale = alloc([128, 4]); b_u = alloc([128, 8]); epst = alloc([128, 1])
        sele = alloc([128, 16, 128], BF16); poolw = alloc([128, 4, 128], BF16); invcnt = alloc([128, 4, 16])
        wr32 = alloc([128, 2, 8, 20]); rbias = alloc([128, 2, 20])
        sqp = [alloc([128, 512], BF16) for _ in range(4)]; rstdp = [alloc([128, 512]) for _ in range(NT)]
        arena_base = state["off"]
        XS_OFF = 57344
        state["off"] = arena_base + XS_OFF
        xs = [alloc([128, D]) for _ in range(4)]
        assert state["off"] <= arena_base + 73728
        state["off"] = arena_base

        ring_state = {"n": 0}

        def ring_load(parts):
            i = ring_state["n"] % NSLOT; ring_state["n"] += 1
            g = P.new_group("ring%d" % i)
            for dst_fn, src in parts:
                dst = dst_fn(ring[i])
                P.add("pool", lambda e, dst=dst, src=src: e.dma_start(out=dst, in_=src), writes=[("ring", i)], grp=g)
            return i

        gc = P.new_group("const")

        gcp = P.new_group("constp")

        def cload(dst, src, eng="sync", key=None):
            P.add(eng, lambda e, dst=dst, src=src: e.dma_start(out=dst, in_=src), writes=[key], grp=(gc if eng == "sync" else gcp))

        cload(ident, ident_d[:, :], key="ident")
        cload(gains.rearrange("p a b -> p (a b)"), gains_d[:, :], key="gains")
        cload(pscale, pscale_d[:, :], key="pscale")
        cload(b_u, bu_d[:, :], key="b_u")
        cload(invcnt.rearrange("p a b -> p (a b)"), invcnt_d[:, :], key="invcnt")
        cload(wr32.rearrange("p l k n -> p l (k n)")[:, 0, :], wr_d[0].rearrange("(p kc) n -> p (kc n)", kc=8), key="wr32")
        cload(wr32.rearrange("p l k n -> p l (k n)")[:, 1, :], wr_d[1].rearrange("(p kc) n -> p (kc n)", kc=8), key="wr32")
        cload(rbias.rearrange("p a b -> p (a b)"), rb_d.partition_broadcast(128), key="rbias")
        cload(sele.rearrange("p a b -> p (a b)")[0:32, :], sele_d[:, :], eng="pool", key="sele")
        cload(poolw, pool_w.rearrange("g c d -> c g d"), eng="pool", key="poolw")
        state["off"] = arena_base
        btab = alloc([128, 8, 256]); r0tab = alloc([128, 8, 256]); emask = alloc([128, 256])
        cload(btab.rearrange("p a b -> p (a b)"), btab_d[:, :], key="btab")
        cload(r0tab.rearrange("p a b -> p (a b)"), r0tab_d[:, :], key="r0tab")
        cload(emask, emask_d[:, :], key="emask")
        P.add("dve", lambda e: e.memset(ones_bf, 1.0), writes=["ones"])
        P.add("dve", lambda e: e.memset(epst, EPS), writes=["eps"])
        P.add("dve", lambda e: e.memset(vtail.rearrange("p a b c -> p (a b c)"), 1.0), writes=["vtail"])
        P.add("dve", lambda e: e.tensor_tensor(out=btab, in0=btab, in1=r0tab, op=ALU.subtract),
              reads=["btab", "r0tab"], writes=["btab"])
        P.add("act", lambda e: e.activation(out=btab, in_=btab, func=AF.Exp), reads=["btab"], writes=["btab"])
        P.add("dve", lambda e: e.tensor_tensor(out=etab, in0=btab, in1=emask.unsqueeze(1).broadcast_to([128, 8, 256]),
                                               op=ALU.mult), reads=["btab", "emask"], writes=["etab"])
        for l in range(2):
            for kc in range(8):
                P.add("dve", lambda e, l=l, kc=kc: e.tensor_scalar(out=wr32[:, l, kc, :], in0=wr32[:, l, kc, :],
                                                                  scalar1=gains[:, 1 + 2 * l, kc:kc + 1], scalar2=None, op0=ALU.mult),
                      reads=["wr32", "gains"], writes=["wr32"])
        P.barrier()

        HK = lambda kc, tt: ("h", kc, tt)
        YK = lambda kc, tt: ("y", kc, tt)

        def norm_stats(tt):
            ts_ = slice(tt * 512, (tt + 1) * 512)
            b = nextbank()
            for kc in range(8):
                P.add("act", lambda e, kc=kc, ts_=ts_: e.activation(out=sqp[kc % 4], in_=h[:, kc, ts_], func=AF.Square),
                      reads=[HK(kc, tt)], writes=[("sq", kc % 4)])
                P.add("pe", lambda e, kc=kc, b=b: e.matmul(bank(b), lhsT=ones_bf, rhs=sqp[kc % 4], start=(kc == 0), stop=(kc == 7)),
                      reads=[("sq", kc % 4), "ones"], writes=[("ps", b)])
            P.add("act", lambda e, b=b, tt=tt: e.activation(out=rstdp[tt], in_=bank(b), func=AF.Ln, scale=1.0 / D, bias=epst[:, 0:1]),
                  reads=[("ps", b), "eps"], writes=[("rstd", tt)])
            P.add("act", lambda e, tt=tt: e.activation(out=rstdp[tt], in_=rstdp[tt], func=AF.Exp, scale=-0.5),
                  reads=[("rstd", tt)], writes=[("rstd", tt)])

        def rmsnorm(gi, ym, out_dtype_note=None):
            for tt in range(NT):
                ts_ = slice(tt * 512, (tt + 1) * 512)
                for kc in range(8):
                    P.add("dve", lambda e, kc=kc, ts_=ts_, tt=tt: e.scalar_tensor_tensor(
                        out=ym[:, kc, ts_], in0=h[:, kc, ts_], scalar=gains[:, gi, kc:kc + 1], in1=rstdp[tt],
                        op0=ALU.mult, op1=ALU.mult), reads=[HK(kc, tt), ("rstd", tt), "gains"], writes=[YK(kc, tt)])
            return rstdp

        def proj_to_h(wslots, src):
            for tt in range(NT):
                ts_ = slice(tt * 512, (tt + 1) * 512)
                for kco in range(8):
                    b = nextbank()
                    for kc in range(8):
                        w = ring[wslots[kc // 4]].rearrange("p (a b) -> p a b", a=4)
                        P.add("pe", lambda e, w=w, kc=kc, kco=kco, b=b, ts_=ts_: e.matmul(
                            bank(b), lhsT=w[:, kc % 4, kco:1024:8], rhs=src[:, kc, ts_], start=(kc == 0), stop=(kc == 7)),
                            reads=[("ring", wslots[kc // 4]), YK(kc, tt)], writes=[("ps", b)])
                    P.add("dve", lambda e, kco=kco, b=b, ts_=ts_: e.tensor_tensor(out=h[:, kco, ts_], in0=bank(b), in1=h[:, kco, ts_], op=ALU.add),
                          reads=[("ps", b), HK(kco, tt)], writes=[HK(kco, tt)])
                norm_stats(tt)

        def load_w_out(wd):
            s = []
            for half in range(2):
                src = wd.rearrange("(kc p) n -> p kc n", p=128)[:, half * 4:(half + 1) * 4, :]
                s.append(ring_load([(lambda r: r.rearrange("p (a b) -> p a b", a=4), src)]))
            return s

        def load_w_in(wd, order=(0, 1, 2, 3)):
            s = [None] * 4
            for q in order:
                src = wd.rearrange("(p kc) n -> p kc n", kc=8)[:, :, q * 512:(q + 1) * 512]
                s[q] = ring_load([(lambda r: r.rearrange("p (a b) -> p a b", a=8), src)])
            return s

        SGU_C_OFF = 73728

        def sgu_const_views():
            o = state["off"]; state["off"] = arena_base + SGU_C_OFF
            v = (alloc([128, 1024]), alloc([128, 8, 128]), alloc([128, 8, 128]), alloc([128, 128]), alloc([128, 2, 8]))
            state["off"] = o
            return v

        def sgu_const_load():
            bvB, bsB, wsT32, trim, lnfm = sgu_const_views()
            gsc = P.new_group("sguc")
            for dst, src, k in ((bvB, bv_d.partition_broadcast(128), "bvB"),
                                (bsB.rearrange("p a b -> p (a b)"), bs_d.partition_broadcast(128), "bsB"),
                                (wsT32.rearrange("p a b -> p (a b)"), wsT_d[:, :], "wsT32"), (trim, trimask_d[:, :], "trim"),
                                (lnfm.rearrange("p a b -> p (a b)"), lnfm_d[:, :], "lnfm")):
                P.add("sync", lambda e, dst=dst, src=src: e.dma_start(out=dst, in_=src), writes=[k], grp=gsc)

        def moe_phase(l):
            state["off"] = arena_base
            if l == 0:
                sgu_const_load()
            tT = alloc([128, 8, TS], BF16)
            rstd = rmsnorm(1 + 2 * l, tT)
            Lb = nextbank(); Tb = nextbank()
            for blk in range(NBLK):
                tt = blk // 4
                for kc in range(8):
                    P.add("pe", lambda e, blk=blk, kc=kc: e.matmul(bank(Lb)[:, blk * 20:(blk + 1) * 20], lhsT=h[:, kc, blk * 128:(blk + 1) * 128],
                                                                  rhs=wr32[:, l, kc, :], start=(kc == 0), stop=(kc == 7)),
                          reads=[HK(kc, tt), "wr32"], writes=[("ps", Lb)])
                P.add("pe", lambda e, blk=blk, tt=tt: e.transpose(bank(Tb)[:, blk * 32:(blk + 1) * 32], rstd[tt][0:32, (blk % 4) * 128:(blk % 4 + 1) * 128], ident[0:32, 0:32]),
                      reads=[("rstd", tt), "ident"], writes=[("ps", Tb)])
            Ls = alloc([128, 8, 20]); rt = alloc([128, 8]); t84 = [alloc([128, 8, 4]) for _ in range(6)]; t8 = [alloc([128, 8]) for _ in range(6)]
            t844 = alloc([128, 8, 4, 4]); chl = alloc([128, 8, 32]); cbf = alloc([128, 8, 16], BF16); cT = alloc([128, TS], BF16)
            R = "rt_scratch"

            def dv(fn, reads=(), writes=()):
                P.add("dve", fn, reads=[R] + list(reads), writes=[R] + list(writes))

            def bc(a):
                return a.unsqueeze(2).broadcast_to([128, 8, 4])

            dv(lambda e: e.tensor_copy(rt, bank(Tb)[:, 0:256:32]), reads=[("ps", Tb)])
            dv(lambda e: e.tensor_tensor(out=Ls, in0=bank(Lb)[:, 0:160].rearrange("p (a b) -> p a b", a=8),
                                         in1=rt.unsqueeze(2).broadcast_to([128, 8, 20]), op=ALU.mult), reads=[("ps", Lb)])
            dv(lambda e: e.tensor_tensor(out=Ls, in0=Ls, in1=rbias[:, l, :].unsqueeze(1).broadcast_to([128, 8, 20]), op=ALU.add), reads=["rbias"])
            gl = Ls[:, :, 0:4]
            gmax, gs, gw, m1, m2, es_ = t8
            oh, gd, esel, e2, sel, en = t84
            dv(lambda e: e.tensor_reduce(out=gmax, in_=gl, axis=AX.X, op=ALU.max))
            dv(lambda e: e.tensor_tensor(out=oh, in0=gl, in1=bc(gmax), op=ALU.is_ge))
            dv(lambda e: e.tensor_tensor(out=gd, in0=gl, in1=bc(gmax), op=ALU.subtract))
            P.add("act", lambda e: e.activation(out=gd, in_=gd, func=AF.Exp), reads=[R], writes=[R])
            dv(lambda e: e.tensor_reduce(out=gs, in_=gd, axis=AX.X, op=ALU.add))
            dv(lambda e: e.reciprocal(out=gw, in_=gs))
            dv(lambda e: e.tensor_tensor(out=t844, in0=Ls[:, :, 4:20].rearrange("p a (g x) -> p a g x", g=4),
                                         in1=oh.unsqueeze(3).broadcast_to([128, 8, 4, 4]), op=ALU.mult))
            dv(lambda e: e.tensor_reduce(out=esel, in_=t844.rearrange("p a g x -> p a x g"), axis=AX.X, op=ALU.add))
            dv(lambda e: e.tensor_reduce(out=m1, in_=esel, axis=AX.X, op=ALU.max))
            dv(lambda e: e.tensor_tensor(out=sel, in0=esel, in1=bc(m1), op=ALU.is_ge))
            dv(lambda e: e.scalar_tensor_tensor(out=e2, in0=sel, scalar=-1e30, in1=esel, op0=ALU.mult, op1=ALU.add))
            dv(lambda e: e.tensor_reduce(out=m2, in_=e2, axis=AX.X, op=ALU.max))
            dv(lambda e: e.tensor_tensor(out=sel, in0=esel, in1=bc(m2), op=ALU.is_ge))
            dv(lambda e: e.tensor_tensor(out=e2, in0=esel, in1=bc(m1), op=ALU.subtract))
            P.add("act", lambda e: e.activation(out=e2, in_=e2, func=AF.Exp), reads=[R], writes=[R])
            dv(lambda e: e.tensor_tensor(out=en, in0=e2, in1=sel, op=ALU.mult))
            dv(lambda e: e.tensor_reduce(out=es_, in_=en, axis=AX.X, op=ALU.add))
            dv(lambda e: e.reciprocal(out=es_, in_=es_))
            dv(lambda e: e.tensor_tensor(out=es_, in0=es_, in1=gw, op=ALU.mult))
            dv(lambda e: e.tensor_tensor(out=en, in0=en, in1=bc(es_), op=ALU.mult))
            dv(lambda e: e.tensor_tensor(out=t844, in0=oh.unsqueeze(3).broadcast_to([128, 8, 4, 4]),
                                         in1=en.unsqueeze(2).broadcast_to([128, 8, 4, 4]), op=ALU.mult))
            comb = t844.rearrange("p a g x -> p a (g x)")
            dv(lambda e: e.tensor_copy(cbf, comb))
            dv(lambda e: e.tensor_copy(chl[:, :, 0:16], cbf))
            dv(lambda e: e.tensor_tensor(out=chl[:, :, 16:32], in0=comb, in1=chl[:, :, 0:16], op=ALU.subtract))
            def emit_cT():
                for half in range(2):
                    cb_ = nextbank()
                    for b4 in range(4):
                        blk = half * 4 + b4
                        P.add("pe", lambda e, blk=blk, b4=b4, cb_=cb_: e.transpose(bank(cb_)[0:32, b4 * 128:(b4 + 1) * 128], chl[:, blk, :], ident),
                              reads=[R, "ident"], writes=[("ps", cb_)])
                    P.add("act", lambda e, half=half, cb_=cb_: e.activation(out=cT[0:32, half * 512:(half + 1) * 512], in_=bank(cb_)[0:32, :], func=AF.Copy),
                          reads=[("ps", cb_)], writes=[("cT", half)])
            ct_done = [False]
            ssb = [alloc([128, 512]) for _ in range(4)]; cbs = [alloc([128, 512]) for _ in range(2)]
            actT = [alloc([128, 4, 512], BF16) for _ in range(2)]
            cnt = {"s": 0, "c": 0, "a": 0}
            slots = {}

            def pair_loads(pi):
                sl = []
                for el in range(2):
                    e_ = 2 * pi + el
                    sl.append(ring_load([
                        (lambda r: r[:, 0:2048], w_gate[l, e_].rearrange("(p kc) f -> p (kc f)", kc=8)),
                        (lambda r: r[:, 2048:4096], w_up[l, e_].rearrange("(p kc) f -> p (kc f)", kc=8))]))
                sd = ring_load([
                    (lambda r: r[:, 0:2048].rearrange("p (a b) -> p a b", a=2), w_down[l, 2 * pi].rearrange("(fc p) d -> p fc d", p=128)),
                    (lambda r: r[:, 2048:4096].rearrange("p (a b) -> p a b", a=2), w_down[l, 2 * pi + 1].rearrange("(fc p) d -> p fc d", p=128))])
                slots[pi] = (sl, sd)

            def emit_E(pi, tt, el):
                sl, sd = slots[pi]
                ts_ = slice(tt * 512, (tt + 1) * 512)
                ai = (pi * NT + tt) % 2
                e_ = 2 * pi + el
                wgu = ring[sl[el]].rearrange("p (g k f) -> p g k f", g=2, k=8)
                ci = cnt["c"] % 2; cnt["c"] += 1
                early = ct_done[0]

                def emit_cb(e_=e_, ci=ci, ts_=ts_, tt=tt):
                    bcb = nextbank()
                    P.add("pe", lambda e, bcb=bcb: e.matmul(bank(bcb), lhsT=sele[0:32, e_, :], rhs=cT[0:32, ts_], start=True, stop=True),
                          reads=["sele", ("cT", tt)], writes=[("ps", bcb)])
                    P.add("act", lambda e, bcb=bcb: e.activation(out=cbs[ci], in_=bank(bcb), func=AF.Copy),
                          reads=[("ps", bcb)], writes=[("cbs", ci)])

                def emit_mults(fc, si, bu_, ci=ci, ai=ai, el=el):
                    P.add("dve", lambda e: e.tensor_tensor(out=ssb[si], in0=ssb[si], in1=cbs[ci], op=ALU.mult),
                          reads=[("ssb", si), ("cbs", ci)], writes=[("ssb", si)])
                    P.add("dve", lambda e: e.tensor_tensor(out=actT[ai][:, el * 2 + fc, :], in0=bank(bu_), in1=ssb[si], op=ALU.mult),
                          reads=[("ps", bu_), ("ssb", si)], writes=[("actT", ai)])

                if early:
                    emit_cb()
                per_fc = []
                for fc in range(2):
                    bg = nextbank(); bu_ = nextbank(); si = cnt["s"] % 4; cnt["s"] += 1
                    for gi_, bb in ((0, bg), (1, bu_)):
                        for kc in range(8):
                            P.add("pe", lambda e, wgu=wgu, gi_=gi_, kc=kc, fc=fc, bb=bb, ts_=ts_: e.matmul(
                                bank(bb), lhsT=wgu[:, gi_, kc, fc * 128:(fc + 1) * 128], rhs=tT[:, kc, ts_], start=(kc == 0), stop=(kc == 7)),
                                reads=[("ring", sl[el]), YK(kc, tt)], writes=[("ps", bb)])
                    P.add("act", lambda e, bg=bg, si=si: e.activation(out=ssb[si], in_=bank(bg), func=AF.Silu),
                          reads=[("ps", bg)], writes=[("ssb", si)])
                    if early:
                        emit_mults(fc, si, bu_)
                    else:
                        per_fc.append((fc, si, bu_))
                if not early:
                    emit_cT(); ct_done[0] = True
                    emit_cb()
                    for fc, si, bu_ in per_fc:
                        emit_mults(fc, si, bu_)

            def emit_D(pi, tt):
                sl, sd = slots[pi]
                ts_ = slice(tt * 512, (tt + 1) * 512)
                ai = (pi * NT + tt) % 2
                for kco in range(8):
                    b = nextbank()
                    for el in range(2):
                        wd = ring[sd][:, el * 2048:(el + 1) * 2048].rearrange("p (a b) -> p a b", a=2)
                        for fc in range(2):
                            first = (el == 0 and fc == 0); last = (el == 1 and fc == 1)
                            P.add("pe", lambda e, wd=wd, fc=fc, kco=kco, b=b, ai=ai, el=el, first=first, last=last: e.matmul(
                                bank(b), lhsT=wd[:, fc, kco:1024:8], rhs=actT[ai][:, el * 2 + fc, :], start=first, stop=last),
                                reads=[("ring", sd), ("actT", ai)], writes=[("ps", b)])
                    P.add("dve", lambda e, kco=kco, b=b, ts_=ts_: e.tensor_tensor(out=h[:, kco, ts_], in0=bank(b), in1=h[:, kco, ts_], op=ALU.add),
                          reads=[("ps", b), HK(kco, tt)], writes=[HK(kco, tt)])
                if pi == 7:
                    norm_stats(tt)

            assert state["off"] <= arena_base + XS_OFF, state["off"] - arena_base
            pts = [(pi, tt) for pi in range(8) for tt in range(NT)]
            pair_loads(0)
            emit_E(0, 0, 0)
            for k, (pi, tt) in enumerate(pts):
                emit_E(pi, tt, 1)
                if k + 1 < len(pts):
                    pn, tn = pts[k + 1]
                    if tn == 0:
                        pair_loads(pn)
                    emit_E(pn, tn, 0)
                emit_D(pi, tt)
            P.barrier()

        out_groups = []
        x_issued = set()

        def issue_x(st_, blk):
            si = blk % 4
            g = P.new_group("xs%d" % si)
            P.add("sync", lambda e, si=si, blk=blk, st_=st_: e.dma_start(out=xs[si], in_=x[st_ * TS + blk * 128:st_ * TS + (blk + 1) * 128, :]),
                  writes=[("xs", si)], grp=g)
            x_issued.add((st_, blk))
        def do_super(st):
            t0 = st * TS
            for blk in range(NBLK):
                si = blk % 4
                if (st, blk) not in x_issued:
                    issue_x(st, blk)
                tt = blk // 4
                for half in range(2):
                    b = nextbank()
                    for k4 in range(4):
                        kc = half * 4 + k4
                        P.add("pe", lambda e, si=si, kc=kc, k4=k4, b=b: e.transpose(bank(b)[:, k4 * 128:(k4 + 1) * 128], xs[si][:, kc:1024:8], ident),
                              reads=[("xs", si), "ident"], writes=[("ps", b)])
                    eng = "dve" if half == 0 else "act"
                    dst = h[:, half * 4:(half + 1) * 4, blk * 128:(blk + 1) * 128]
                    src = bank(b).rearrange("p (a b) -> p a b", a=4)
                    if eng == "dve":
                        P.add("dve", lambda e, dst=dst, src=src: e.tensor_copy(dst, src), reads=[("ps", b)], writes=[HK(half * 4 + k, tt) for k in range(4)])
                    else:
                        P.add("act", lambda e, dst=dst, src=src: e.activation(out=dst, in_=src, func=AF.Copy), reads=[("ps", b)], writes=[HK(half * 4 + k, tt) for k in range(4)])
                if blk % 4 == 3:
                    norm_stats(blk // 4)
            P.barrier()

            if stop_stage >= 1:
                state["off"] = arena_base
                ym = alloc([128, 8, TS], BF16)
                rmsnorm(0, ym)
                qT = alloc([128, 4, TS], BF16); kTw = alloc([128, 4, 512 + TS], BF16); Vw = alloc([128, 4 + NBLK, 8, 128], BF16)
                pTg = [alloc([128, 16 + TS]) for _ in range(2)]; tA = alloc([128, 16 + TS]); tB = alloc([128, 16 + TS])
                pooled = alloc([128, TS], BF16); mixP = alloc([128, 4, TS], BF16)
                Pb = [alloc([128, 512], BF16) for _ in range(4)]; Rb = [alloc([128, 128]) for _ in range(2)]
                MK = lambda kc, tt: ("mix", kc, tt)
                if st > 0:
                    P.add("dve", lambda e: e.tensor_copy(kTw[:, :, 0:512], ktail), reads=["ktail"], writes=[("kT", j) for j in range(4)])
                    P.add("dve", lambda e: e.tensor_copy(Vw[:, 0:4].rearrange("p a b c -> p (a b c)"), vtail.rearrange("p a b c -> p (a b c)")),
                          reads=["vtail"], writes=[("V", b_) for b_ in range(4)])
                for b_ in range(NBLK):
                    P.add("dve", lambda e, b_=b_: e.memset(Vw[:, 4 + b_, :, 64:128], 1.0), writes=[("V", 4 + b_)])
                wsl = load_w_in(ab_w_in)
                def proj_qk(which, slot_i):
                    for j in range(4):
                        for tt in range(NT):
                            ts_ = slice(tt * 512, (tt + 1) * 512)
                            b = nextbank()
                            w = ring[wsl[slot_i]].rearrange("p (a b) -> p a b", a=8)
                            for kc in range(8):
                                P.add("pe", lambda e, w=w, kc=kc, j=j, b=b, ts_=ts_: e.matmul(bank(b), lhsT=w[:, kc, j * 128:(j + 1) * 128], rhs=ym[:, kc, ts_],
                                                                                         start=(kc == 0), stop=(kc == 7)),
                                      reads=[("ring", wsl[slot_i]), YK(kc, tt)], writes=[("ps", b)])
                            if which == "q":
                                P.add("act", lambda e, j=j, b=b, ts_=ts_: e.activation(out=qT[:, j, ts_], in_=bank(b), func=AF.Copy),
                                      reads=[("ps", b)], writes=[("qT", j)])
                            else:
                                P.add("dve", lambda e, j=j, b=b, tt=tt: e.tensor_copy(kTw[:, j, 512 + tt * 512:512 + (tt + 1) * 512], bank(b)),
                                      reads=[("ps", b)], writes=[("kT", j)])
                def proj_v(blks):
                    wv = ring[wsl[3]].rearrange("p (a b) -> p a b", a=8)
                    for blk in blks:
                        b = nextbank(); tt = blk // 4
                        for kc in range(8):
                            P.add("pe", lambda e, kc=kc, blk=blk, b=b: e.matmul(bank(b), lhsT=ym[:, kc, blk * 128:(blk + 1) * 128], rhs=wv[:, kc, :],
                                                                          start=(kc == 0), stop=(kc == 7)),
                                  reads=[("ring", wsl[3]), YK(kc, tt)], writes=[("ps", b)])
                        eng = "act" if blk % 2 else "dve"
                        dst = Vw[:, 4 + blk, :, 0:64]; src = bank(b).rearrange("p (a b) -> p a b", a=8)
                        if eng == "dve":
                            P.add("dve", lambda e, dst=dst, src=src: e.tensor_copy(dst, src), reads=[("ps", b)], writes=[("V", 4 + blk)])
                        else:
                            P.add("act", lambda e, dst=dst, src=src: e.activation(out=dst, in_=src, func=AF.Copy), reads=[("ps", b)], writes=[("V", 4 + blk)])
                wp = ring[wsl[0]].rearrange("p (a b) -> p a b", a=8)
                def pool_proj(g_):
                    pt = pTg[g_ % 2]; PK = ("pT", g_ % 2)
                    if st > 0:
                        P.add("dve", lambda e, pt=pt, g_=g_: e.tensor_copy(pt[:, 0:16], ptail[:, g_, :]), reads=["ptail"], writes=[PK])
                    else:
                        P.add("dve", lambda e, pt=pt: e.memset(pt[:, 0:16], 0.0), writes=[PK])
                    for tt in range(NT):
                        ts_ = slice(tt * 512, (tt + 1) * 512)
                        b = nextbank()
                        for kc in range(8):
                            P.add("pe", lambda e, kc=kc, g_=g_, b=b, ts_=ts_: e.matmul(bank(b), lhsT=wp[:, kc, g_ * 128:(g_ + 1) * 128], rhs=ym[:, kc, ts_],
                                                                                 start=(kc == 0), stop=(kc == 7)),
                                  reads=[("ring", wsl[0]), YK(kc, tt)], writes=[("ps", b)])
                        P.add("act", lambda e, pt=pt, b=b, tt=tt: e.activation(out=pt[:, 16 + tt * 512:16 + (tt + 1) * 512], in_=bank(b), func=AF.Copy),
                              reads=[("ps", b)], writes=[PK])
                    P.add("dve", lambda e, pt=pt, g_=g_: e.tensor_copy(ptail[:, g_, :], pt[:, TS:TS + 16]), reads=[PK], writes=["ptail"])
                def pool_chain(g_):
                    pt = pTg[g_ % 2]; PK = ("pT", g_ % 2)
                    W_ = 16 + TS
                    srcb = pt; bufs = [tA, tB]; keys = ["tA", "tB"]; sk = PK
                    for lev in range(g_ + 1):
                        sh = 1 << lev; lo = (1 << (lev + 1)) - 1
                        dstb = bufs[lev % 2]; dk = keys[lev % 2]
                        P.add("dve", lambda e, dstb=dstb, srcb=srcb, sh=sh, lo=lo: e.tensor_tensor(out=dstb[:, lo:W_], in0=srcb[:, lo:W_], in1=srcb[:, lo - sh:W_ - sh], op=ALU.add),
                              reads=[sk], writes=[dk])
                        srcb = dstb; sk = dk
                    wdw = float(1 << (g_ + 1))
                    P.add("dve", lambda e, srcb=srcb, pt=pt, wdw=wdw: e.scalar_tensor_tensor(out=pooled, in0=srcb[:, 16:W_], scalar=1.0 / wdw, in1=pt[:, 16:W_],
                                                                                       op0=ALU.mult, op1=ALU.subtract), reads=[sk, PK], writes=["pooled"])
                    if st == 0:
                        P.add("dve", lambda e, srcb=srcb, g_=g_: e.tensor_tensor(out=srcb[:, 0:16], in0=srcb[:, 16:32], in1=invcnt[:, g_, :], op=ALU.mult),
                              reads=[sk, "invcnt"], writes=[sk])
                        P.add("dve", lambda e, srcb=srcb, pt=pt: e.tensor_tensor(out=pooled[:, 0:16], in0=srcb[:, 0:16], in1=pt[:, 16:32], op=ALU.subtract),
                              reads=[sk, PK, "pooled"], writes=["pooled"])
                def pool_mm(g_):
                    for tt in range(NT):
                        ts_ = slice(tt * 512, (tt + 1) * 512)
                        b = nextbank()
                        P.add("pe", lambda e, g_=g_, b=b, ts_=ts_: e.matmul(bank(b), lhsT=poolw[:, g_, :], rhs=pooled[:, ts_], start=True, stop=True),
                              reads=["poolw", "pooled"], writes=[("ps", b)])
                        P.add("dve", lambda e, g_=g_, b=b, ts_=ts_: e.tensor_scalar(out=mixP[:, g_, ts_], in0=bank(b), scalar1=pscale[:, g_:g_ + 1], scalar2=None, op0=ALU.mult),
                              reads=[("ps", b), "pscale"], writes=[MK(g_, tt)])
                pool_proj(0); pool_proj(1); proj_qk("q", 1); pool_chain(0); pool_mm(0); pool_proj(2); proj_qk("k", 2)
                pool_chain(1); pool_mm(1); pool_proj(3); pool_chain(2); proj_v(range(0, NBLK // 2)); pool_mm(2)
                pool_chain(3); proj_v(range(NBLK // 2, NBLK)); pool_mm(3)
                wo = load_w_out(ab_w_out)
                lbmin = 4 if st == 0 else 0
                LA = 3
                osb = [(tA[:, 0:512], "tA"), (tB[:, 0:512], "tB"), (pTg[0][:, 0:512], ("pT", 0)), (pTg[1][:, 0:512], ("pT", 1))]
                Rn = pooled.bitcast(F32)
                items = []
                for hh in range(8):
                    for half in range(2):
                        mr0 = half * 4; mr1 = half * 4 + 3
                        first = True
                        for lb in range(max(lbmin, mr0), mr1 + 5):
                            m_lo = max(lb - 4, mr0); m_hi = min(lb, mr1)
                            if m_lo <= m_hi:
                                items.append((hh, half, lb, m_lo, m_hi, first)); first = False

                def emit_qk(i):
                    hh, half, lb, m_lo, m_hi, first = items[i]
                    j = hh // 2; po = (hh % 2) * 64
                    nq = (m_hi - m_lo + 1) * 128; q0 = m_lo * 128
                    sb = i % 4
                    P.add("pe", lambda e, j=j, po=po, lb=lb, nq=nq, q0=q0, sb=sb: e.matmul(
                        bank(sb)[:, 0:nq], lhsT=kTw[po:po + 64, j, lb * 128:(lb + 1) * 128], rhs=qT[po:po + 64, j, q0:q0 + nq], start=True, stop=True),
                        reads=[("kT", j), ("qT", j)], writes=[("ps", sb)])

                def emit_rest(i, deferred):
                    hh, half, lb, m_lo, m_hi, first = items[i]
                    mr1_ = half * 4 + 3
                    j = hh // 2; po = (hh % 2) * 64
                    ob = 4 + (hh * 2 + half) % 4
                    OT = bank(ob)
                    if first:
                        P.add("dve", lambda e, OT=OT: e.memset(OT, 0.0), writes=[("ps", ob)])
                    nq = (m_hi - m_lo + 1) * 128
                    sb = i % 4; pi_ = i % 4
                    P.add("act", lambda e, pi_=pi_, sb=sb, nq=nq: e.activation(out=Pb[pi_][:, 0:nq], in_=bank(sb)[:, 0:nq], func=AF.Exp, scale=ATT_SCALE),
                          reads=[("ps", sb)], writes=[("Pb", pi_)])
                    has0 = (m_lo == lb - 4); has1 = (m_lo <= lb - 3 <= m_hi)
                    if has0 and has1:
                        pc, ec = 0, (0, 256)
                    elif has0:
                        pc, ec = 0, (0, 128)
                    elif has1:
                        pc, ec = (lb - 3 - m_lo) * 128, (128, 256)
                    else:
                        pc = None
                    if pc is not None:
                        P.add("dve", lambda e, pi_=pi_, pc=pc, ec=ec, hh=hh: e.tensor_tensor(out=Pb[pi_][:, pc:pc + ec[1] - ec[0]], in0=Pb[pi_][:, pc:pc + ec[1] - ec[0]],
                                                                                       in1=etab[:, hh, ec[0]:ec[1]], op=ALU.mult),
                              reads=[("Pb", pi_), "etab"], writes=[("Pb", pi_)])
                    while deferred and deferred[0][0] <= i - 2:
                        deferred.pop(0)[1]()
                    vfull = Vw[:, lb, hh, :]
                    vhalf = Vw[64:128, lb, hh, :]
                    m = m_lo
                    while m <= m_hi:
                        last = (lb == m + 4); nat = (lb == m)
                        pc0 = (m - m_lo) * 128; oc = (m - half * 4) * 128
                        if nat:
                            P.add("pe", lambda e, oc=oc, pc0=pc0, pi_=pi_, OT=OT, vfull=vfull, last=last: e.matmul(
                                OT[:, oc:oc + 64], lhsT=vfull, rhs=Pb[pi_][:, pc0:pc0 + 64], start=False, stop=last, skip_group_check=True),
                                reads=[("V", lb), ("Pb", pi_)], writes=[("ps", ob)])
                            P.add("pe", lambda e, oc=oc, pc0=pc0, pi_=pi_, OT=OT, vhalf=vhalf, last=last: e.matmul(
                                OT[:, oc + 64:oc + 128], lhsT=vhalf, rhs=Pb[pi_][64:128, pc0 + 64:pc0 + 128], start=False, stop=last, skip_group_check=True),
                                reads=[("V", lb), ("Pb", pi_)], writes=[("ps", ob)])
                            m2_ = m + 1
                        else:
                            m2_ = m + 1
                            while (m2_ <= m_hi and (lb == m2_ + 4) == last and lb != m2_):
                                m2_ += 1
                            nn = (m2_ - m) * 128
                            P.add("pe", lambda e, oc=oc, nn=nn, pc0=pc0, pi_=pi_, OT=OT, vfull=vfull, last=last: e.matmul(
                                OT[:, oc:oc + nn], lhsT=vfull, rhs=Pb[pi_][:, pc0:pc0 + nn], start=False, stop=last, skip_group_check=True),
                                reads=[("V", lb), ("Pb", pi_)], writes=[("ps", ob)])
                        if lb == mr1_ + 4 and m2_ > mr1_:
                            u_ = hh * 2 + half
                            OS, osk = osb[u_ % 4]

                            def norm_(OS=OS, osk=osk, OT=OT, j=j, po=po, ob=ob, half=half):
                                P.add("dve", lambda e: e.tensor_copy(OS, OT), reads=[("ps", ob)], writes=[osk])
                                P.add("act", lambda e: e.activation(out=Rn[0:64, :], in_=OS[64:128, :], func=AF.Ln), reads=[osk], writes=["pooled"])
                                P.add("act", lambda e: e.activation(out=Rn[0:64, :], in_=Rn[0:64, :], func=AF.Exp, scale=-1.0), reads=["pooled"], writes=["pooled"])
                                P.add("dve", lambda e: e.tensor_tensor(out=ym[po:po + 64, j, half * 512:(half + 1) * 512], in0=OS[0:64, :], in1=Rn[0:64, :], op=ALU.mult),
                                      reads=[osk, "pooled"], writes=[YK(j, half)])
                            deferred.append((i, norm_))
                        m = m2_

                for i in range(min(LA, len(items))):
                    emit_qk(i)
                deferred = []
                for i in range(len(items)):
                    if i + LA < len(items):
                        emit_qk(i + LA)
                    emit_rest(i, deferred)
                for _, fn_ in deferred:
                    fn_()
                P.add("dve", lambda e: e.tensor_copy(ktail, kTw[:, :, TS:TS + 512]), reads=[("kT", j) for j in range(4)], writes=["ktail"])
                P.add("dve", lambda e: e.tensor_copy(vtail.rearrange("p a b c -> p (a b c)"), Vw[:, NBLK:NBLK + 4].rearrange("p a b c -> p (a b c)")),
                      reads=[("V", b_) for b_ in range(NBLK, NBLK + 4)], writes=["vtail"])
                for tt in range(NT):
                    ts_ = slice(tt * 512, (tt + 1) * 512)
                    for kco in range(8):
                        b = nextbank()
                        for kc in range(8):
                            w = ring[wo[kc // 4]].rearrange("p (a b) -> p a b", a=4)
                            srcm = mixP[:, kc, ts_] if kc < 4 else ym[:, kc - 4, ts_]
                            P.add("pe", lambda e, w=w, kc=kc, kco=kco, b=b, srcm=srcm: e.matmul(bank(b), lhsT=w[:, kc % 4, kco:1024:8], rhs=srcm,
                                                                                         start=(kc == 0), stop=(kc == 7)),
                                  reads=[("ring", wo[kc // 4]), MK(kc, tt) if kc < 4 else YK(kc - 4, tt)], writes=[("ps", b)])
                        P.add("dve", lambda e, kco=kco, b=b, ts_=ts_: e.tensor_tensor(out=h[:, kco, ts_], in0=bank(b), in1=h[:, kco, ts_], op=ALU.add),
                              reads=[("ps", b), HK(kco, tt)], writes=[HK(kco, tt)])
                    norm_stats(tt)
                P.barrier()
            if stop_stage >= 2:
                moe_phase(0)
            if stop_stage >= 3:
                state["off"] = arena_base
                ym = alloc([128, 8, TS], BF16)
                bvB, bsB, wsT32, trim, lnfm = sgu_const_views()
                Cb = alloc([128, 8, 128]); wsT = alloc([128, 8, 128], BF16)
                P.add("dve", lambda e: e.tensor_tensor(out=wsT, in0=wsT32, in1=trim.unsqueeze(1).broadcast_to([128, 8, 128]), op=ALU.mult),
                      reads=["wsT32", "trim"], writes=["wsT"])
                for half in range(2):
                    b = nextbank()
                    P.add("pe", lambda e, half=half, b=b: e.matmul(bank(b), lhsT=ones_bf, rhs=wsT.rearrange("p a b -> p (a b)")[:, half * 512:(half + 1) * 512], start=True, stop=True),
                          reads=["wsT", "ones"], writes=[("ps", b)])
                    for h4 in range(4):
                        hh = half * 4 + h4
                        P.add("dve", lambda e, hh=hh, h4=h4, b=b: e.scalar_tensor_tensor(out=Cb[:, hh, :], in0=bank(b)[:, h4 * 128:(h4 + 1) * 128], scalar=lnfm[:, 1, hh:hh + 1],
                                                                                   in1=bsB[:, hh, :], op0=ALU.mult, op1=ALU.add),
                              reads=[("ps", b), "lnfm", "bsB"], writes=["Cb"])
                rmsnorm(2, ym)
                uT = alloc([128, 8, TS], BF16); vn = alloc([128, NBLK, 1024], BF16)
                vpre = [alloc([128, 1024]) for _ in range(2)]; tmp5 = [alloc([128, 512]) for _ in range(2)]
                stats = alloc([128, NBLK, 2, 6]); mv = alloc([128, NBLK, 2]); lrs = alloc([128, NBLK]); nmean = alloc([128, NBLK])
                assert state["off"] <= arena_base + SGU_C_OFF, state["off"] - arena_base
                wsl = load_w_in(sgu_w_in, order=(2, 3, 0, 1))
                for blk in range(NBLK):
                    tt = blk // 4; vi = blk % 2
                    for half in range(2):
                        w = ring[wsl[2 + half]].rearrange("p (a b) -> p a b", a=8)
                        b = nextbank()
                        for kc in range(8):
                            P.add("pe", lambda e, w=w, kc=kc, blk=blk, b=b: e.matmul(bank(b), lhsT=ym[:, kc, blk * 128:(blk + 1) * 128], rhs=w[:, kc, :],
                                                                               start=(kc == 0), stop=(kc == 7)),
                                  reads=[("ring", wsl[2 + half]), YK(kc, tt)], writes=[("ps", b)])
                        P.add("dve", lambda e, vi=vi, half=half, b=b: e.tensor_tensor(out=vpre[vi][:, half * 512:(half + 1) * 512], in0=bank(b),
                                                                                  in1=bvB[:, half * 512:(half + 1) * 512], op=ALU.add),
                              reads=[("ps", b), "bvB"], writes=[("vpre", vi)])
                    P.add("act", lambda e, vi=vi, blk=blk: e.activation(out=vn[:, blk, :], in_=vpre[vi], func=AF.Gelu), reads=[("vpre", vi)], writes=[("vn", blk)])
                    for half in range(2):
                        P.add("dve", lambda e, half=half, blk=blk: e.bn_stats(out=stats[:, blk, half, :], in_=vn[:, blk, half * 512:(half + 1) * 512]),
                              reads=[("vn", blk)], writes=["stats"])
                    P.add("dve", lambda e, blk=blk: e.bn_aggr(out=mv[:, blk, :], in_=stats[:, blk, :, :]), reads=["stats"], writes=["mv"])
                for c in range(8):
                    w = ring[wsl[c // 4]].rearrange("p (a b) -> p a b", a=8)
                    for tt in range(NT):
                        ts_ = slice(tt * 512, (tt + 1) * 512)
                        b = nextbank()
                        for kc in range(8):
                            P.add("pe", lambda e, w=w, kc=kc, c=c, b=b, ts_=ts_: e.matmul(bank(b), lhsT=w[:, kc, (c % 4) * 128:(c % 4 + 1) * 128], rhs=ym[:, kc, ts_],
                                                                                     start=(kc == 0), stop=(kc == 7)),
                                  reads=[("ring", wsl[c // 4]), YK(kc, tt)], writes=[("ps", b)])
                        P.add("act", lambda e, c=c, b=b, ts_=ts_: e.activation(out=uT[:, c, ts_], in_=bank(b), func=AF.Gelu, bias=b_u[:, c:c + 1]),
                              reads=[("ps", b), "b_u"], writes=[("uT", c, tt)])
                P.add("act", lambda e: e.activation(out=lrs, in_=mv[:, :, 1], func=AF.Ln, bias=epst[:, 0:1]), reads=["mv", "eps"], writes=["lrs"])
                P.add("act", lambda e: e.activation(out=lrs, in_=lrs, func=AF.Exp, scale=-0.5), reads=["lrs"], writes=["lrs"])
                wo = load_w_out(sgu_w_out)
                P.add("dve", lambda e: e.scalar_tensor_tensor(out=nmean, in0=mv[:, :, 0], scalar=-1.0, in1=lrs, op0=ALU.mult, op1=ALU.mult),
                      reads=["mv", "lrs"], writes=["nmean"])
                for blk in range(NBLK):
                    P.add("act", lambda e, blk=blk: e.activation(out=vn[:, blk, :], in_=vn[:, blk, :], func=AF.Identity, scale=lrs[:, blk:blk + 1], bias=nmean[:, blk:blk + 1]),
                          reads=[("vn", blk), "nmean", "lrs"], writes=[("vn", blk)])
                for half in range(NBLK // 4):
                    for hh in range(8):
                        b = nextbank(); ti = hh % 2
                        for b4 in range(4):
                            blk = half * 4 + b4
                            P.add("pe", lambda e, hh=hh, blk=blk, b4=b4, b=b: e.matmul(bank(b)[:, b4 * 128:(b4 + 1) * 128], lhsT=vn[:, blk, hh * 128:(hh + 1) * 128],
                                                                                  rhs=wsT[:, hh, :], start=True, stop=True),
                                  reads=[("vn", blk), "wsT"], writes=[("ps", b)])
                        P.add("dve", lambda e, hh=hh, b=b, ti=ti: e.scalar_tensor_tensor(out=tmp5[ti].rearrange("p (a b) -> p a b", a=4), in0=bank(b).rearrange("p (a b) -> p a b", a=4),
                                                                                    scalar=lnfm[:, 0, hh:hh + 1], in1=Cb[:, hh, :].unsqueeze(1).broadcast_to([128, 4, 128]),
                                                                                    op0=ALU.mult, op1=ALU.add),
                              reads=[("ps", b), "Cb", "lnfm"], writes=[("tmp5", ti)])
                        P.add("dve", lambda e, hh=hh, half=half, ti=ti: e.tensor_tensor(out=ym[:, hh, half * 512:(half + 1) * 512], in0=tmp5[ti],
                                                                                   in1=uT[:, hh, half * 512:(half + 1) * 512], op=ALU.mult),
                              reads=[("tmp5", ti), ("uT", hh, half)], writes=[YK(hh, half)])
                proj_to_h(wo, ym)
                P.barrier()
            if stop_stage >= 4:
                moe_phase(1)
            state["off"] = arena_base
            yf = alloc([128, 8, TS])
            if st + 1 < n_super:
                issue_x(st + 1, 0); issue_x(st + 1, 1); issue_x(st + 1, 2); issue_x(st + 1, 3)
            if stop_stage >= 5:
                rmsnorm(4, yf)
                src_t = yf; SK = YK
            else:
                src_t = h; SK = HK
            osl = [alloc([128, D]) for _ in range(4)]
            assert state["off"] <= arena_base + XS_OFF, state["off"] - arena_base
            for blk in range(NBLK):
                si = blk % 4; tt = blk // 4
                ov = osl[si].rearrange("t (p kc) -> t kc p", kc=8)
                for half in range(2):
                    b = nextbank()
                    for k4 in range(4):
                        kc = half * 4 + k4
                        P.add("pe", lambda e, kc=kc, k4=k4, b=b, blk=blk: e.transpose(bank(b)[:, k4 * 128:(k4 + 1) * 128], src_t[:, kc, blk * 128:(blk + 1) * 128], ident),
                              reads=[SK(kc, tt), "ident"], writes=[("ps", b)])
                    dst = ov[:, half * 4:(half + 1) * 4, :]; src = bank(b).rearrange("p (a b) -> p a b", a=4)
                    P.add("act", lambda e, dst=dst, src=src: e.activation(out=dst, in_=src, func=AF.Copy), reads=[("ps", b)], writes=[("os", si)])
                g = P.new_group("os%d" % si)
                P.add("sync", lambda e, si=si, blk=blk, t0=t0: e.dma_start(out=out[t0 + blk * 128:t0 + (blk + 1) * 128, :], in_=osl[si]),
                      reads=[("os", si)], writes=[], grp=g)
                out_groups.append(g)
            if st == n_super - 1:
                P.barrier()
        for st_ in range(n_super):
            do_super(st_)
        P.final_tokens = out_groups[-4:]
        P.emit(nc, es)
    return nc


def _host_consts(inp):
    c = {}
    f = lambda a: np.ascontiguousarray(a, dtype=np.float32)
    gains = np.stack([inp["norm_mix_g"][0], inp["norm_ffn_g"][0], inp["norm_mix_g"][1], inp["norm_ffn_g"][1], inp["final_norm_g"]], 0)
    c["gains"] = f(gains.reshape(5, 128, 8).transpose(1, 0, 2).reshape(128, 40))
    c["ab_w_in"] = f(inp["ab_w_in"][0]); c["pool_w"] = f(inp["pool_w"][0])
    c["pscale"] = f(inp["pool_scale"][0].reshape(4, 128).T)
    rb = inp["att_rel_bias"][0]
    p = np.arange(128)[:, None, None]; i = np.arange(64)[None, None, :]
    order = [0, 1, 2, 3]
    idx = np.concatenate([np.clip(p - 64 * d - i, -128, 128) + 128 for d in order], axis=2)
    idx = np.broadcast_to(idx, (128, 8, 256))
    hh = np.arange(8)[None, :, None]
    c["btab"] = f(rb[hh, idx].reshape(128, 2048))
    c["r0tab"] = f(np.broadcast_to(rb[:, 0][None, :, None], (128, 8, 256)).reshape(128, 2048))
    em = np.ones((128, 256), np.float32); em[64:, 0:64] = 0.0
    c["emask"] = em
    c["ab_w_out"] = f(inp["ab_w_out"][0]); c["sgu_w_in"] = f(inp["sgu_w_in"][0])
    c["bu"] = f(inp["sgu_b_in"][0][:1024].reshape(8, 128).T); c["bv"] = f(inp["sgu_b_in"][0][1024:])
    c["lnfm"] = f(np.stack([inp["sgu_ln_g"][0].reshape(8, 128).T, inp["sgu_ln_b"][0].reshape(8, 128).T], axis=1).reshape(128, 16))
    c["wsT"] = f(inp["sgu_w_s"][0].transpose(2, 0, 1).reshape(128, 1024))
    c["trimask"] = f(np.triu(np.ones((128, 128), np.float32)))
    c["bs"] = f(inp["sgu_b_s"][0].reshape(1024)); c["sgu_w_out"] = f(inp["sgu_w_out"][0])
    wr = [np.concatenate([inp["moe_wg_router"][l], inp["moe_we_router"][l].transpose(1, 0, 2).reshape(D, 16)], axis=1) for l in range(2)]
    c["wr"] = f(np.stack(wr, 0))
    c["rbias"] = f(np.concatenate([np.concatenate([inp["moe_bg_router"][l], inp["moe_be_router"][l].reshape(16)]) for l in range(2)]))
    c["w_gate"] = f(inp["moe_w_gate"]); c["w_up"] = f(inp["moe_w_up"]); c["w_down"] = f(inp["moe_w_down"])
    c["ident"] = np.eye(128, dtype=np.float32)
    se = np.zeros((32, 16, 128), np.float32)
    for e in range(16):
        se[e, e, :] = 1.0; se[16 + e, e, :] = 1.0
    c["sele"] = se.reshape(32, 2048)
    ic = np.zeros((128, 4, 16), np.float32)
    for g in range(4):
        w = 2 ** (g + 1)
        ic[:, g, :] = 1.0 / np.minimum(np.arange(16) + 1, w)
    c["invcnt"] = ic.reshape(128, 64)
    return c


_NC_CACHE = {}


def kernel(**inputs):
    inp = {k: np.asarray(v) for k, v in inputs.items()}
    consts = _host_consts(inp)
    if "nc" not in _NC_CACHE:
        _NC_CACHE["nc"] = build_program()
    nc = _NC_CACHE["nc"]
    xin = np.ascontiguousarray(inp["x"], dtype=np.float32)
    in_maps = []
    for b in range(8):
        m = dict(consts); m["x"] = xin[b]
        in_maps.append(m)
    res = run_bass_kernel_spmd(nc, in_maps, core_ids=list(range(8)))
    return np.stack([np.asarray(r["out"], dtype=np.float32) for r in res.results], axis=0)
```

```python
import numpy as np
from contextlib import ExitStack
import concourse.bass as bass
import concourse.mybir as mybir
from concourse.bass_utils import run_bass_kernel_spmd

F32 = mybir.dt.float32
BF16 = mybir.dt.bfloat16
AF = mybir.ActivationFunctionType
ALU = mybir.AluOpType
AX = mybir.AxisListType

D = 1024
SEQ = 4096
TS = 1024
NT = TS // 512
NBLK = TS // 128
EPS = 1e-6
ATT_SCALE = 64 ** -0.5
ENGS = ["sync", "act", "pool", "dve", "pe"]


class Op:
    __slots__ = ("eng", "fn", "deps", "sig", "sigidx", "grp", "is_dma")

    def __init__(self, eng, fn, is_dma=False, grp=None):
        self.eng = eng; self.fn = fn; self.deps = set(); self.sig = False
        self.sigidx = None; self.grp = grp; self.is_dma = is_dma


class Grp:
    def __init__(self, sem):
        self.sem = sem; self.val = 0


class Prog:
    def __init__(self):
        self.ops = {e: [] for e in ENGS}
        self.tiles = {}
        self.dma_cnt = {}
        self.pending = {e: set() for e in ENGS}
        self.final_tokens = []

    def new_group(self, sem):
        g = Grp(sem)
        g.val = self.dma_cnt.get(sem, 0)
        return g

    def add(self, eng, fn, reads=(), writes=(), grp=None):
        op = Op(eng, fn, is_dma=grp is not None, grp=grp)
        if grp is not None:
            self.dma_cnt[grp.sem] = self.dma_cnt.get(grp.sem, 0) + 16
            grp.val = self.dma_cnt[grp.sem]
        raw = set(); oth = set()
        for k in reads:
            st = self.tiles.get(k)
            if st is not None and st[0] is not None:
                raw.add(st[0])
        for k in writes:
            st = self.tiles.get(k)
            if st is not None:
                if st[0] is not None:
                    oth.add(st[0])
                oth.update(st[1].values())
        for d in raw | oth:
            if d is op:
                continue
            if d.grp is not None and d.grp is op.grp:
                continue
            if d.is_dma or op.is_dma or d.eng != eng:
                op.deps.add(d)
            elif eng != "pe":
                op.deps.add(d)
        for d in self.pending[eng]:
            op.deps.add(d)
        self.pending[eng] = set()
        for k in reads:
            self.tiles.setdefault(k, [None, {}])[1][eng if not op.is_dma else id(op)] = op
        for k in writes:
            self.tiles[k] = [op, {}]
        self.ops[eng].append(op)
        return op

    def barrier(self, engs=("sync", "act", "dve", "pe")):
        lasts = {e: self.ops[e][-1] for e in engs if self.ops[e]}
        n0 = getattr(self, "_sync_mark", 0)
        dmas = [o for o in self.ops["sync"][n0:] if o.is_dma]
        self._sync_mark = len(self.ops["sync"])
        for e in engs:
            for e2, o in lasts.items():
                if e2 != e:
                    self.pending[e].add(o)
            for o in dmas:
                self.pending[e].add(o)

    def emit(self, nc, es):
        for e in ENGS:
            for op in self.ops[e]:
                for d in op.deps:
                    if not d.is_dma:
                        d.sig = True
        for e in ENGS:
            n = 0
            for op in self.ops[e]:
                if op.sig and not op.is_dma:
                    n += 1; op.sigidx = n
        sems = {e: es.enter_context(nc.semaphore("s_" + e)) for e in ENGS}
        dsems = {}
        for e in ENGS:
            for op in self.ops[e]:
                if op.is_dma and op.grp.sem not in dsems:
                    dsems[op.grp.sem] = es.enter_context(nc.semaphore("d_" + op.grp.sem))
        block = es.enter_context(nc.Block())
        final_tokens = self.final_tokens

        def run(e, eng):
            waited = {}
            for op in self.ops[e]:
                need = {}
                for d in op.deps:
                    if d.is_dma:
                        s, v = dsems[d.grp.sem], d.grp.val
                    else:
                        s, v = sems[d.eng], d.sigidx
                    key = id(s)
                    if waited.get(key, 0) < v:
                        if key not in need or need[key][1] < v:
                            need[key] = (s, v)
                for key, (s, v) in need.items():
                    eng.wait_ge(s, v); waited[key] = v
                inst = op.fn(eng)
                if op.is_dma:
                    inst.then_inc(dsems[op.grp.sem], 16)
                elif op.sig:
                    inst.then_inc(sems[e], 1)
            if e == "sync":
                for g in final_tokens:
                    eng.wait_ge(dsems[g.sem], g.val)

        block.sync(lambda eng: run("sync", eng))
        block.scalar(lambda eng: run("act", eng))
        block.gpsimd(lambda eng: run("pool", eng))
        block.vector(lambda eng: run("dve", eng))
        block.tensor(lambda eng: run("pe", eng))


FAST_RECIP = False


def RECIP(e, out, in_):
    if FAST_RECIP:
        return e.reciprocal_approx_fast(out=out, in_=in_)
    return e.reciprocal(out=out, in_=in_)


def build_program(n_super=SEQ // TS, stop_stage=99):
    nc = bass.Bass("TRN2", target_bir_lowering=False)
    P = Prog()

    def din(name, shape):
        return nc.dram_tensor(name, list(shape), F32, kind="ExternalInput").ap()

    x = din("x", [SEQ, D])
    out = nc.dram_tensor("out", [SEQ, D], F32, kind="ExternalOutput").ap()
    gains_d = din("gains", [128, 5 * 8])
    ab_w_in = din("ab_w_in", [D, 2048]); pool_w = din("pool_w", [4, 128, 128])
    pscale_d = din("pscale", [128, 4]); btab_d = din("btab", [128, 8 * 256]); r0tab_d = din("r0tab", [128, 8 * 256])
    emask_d = din("emask", [128, 256]); ab_w_out = din("ab_w_out", [D, D])
    sgu_w_in = din("sgu_w_in", [D, 2048]); bu_d = din("bu", [128, 8]); bv_d = din("bv", [1024])
    lnfm_d = din("lnfm", [128, 16]); wsT_d = din("wsT", [128, 1024])
    trimask_d = din("trimask", [128, 128]); bs_d = din("bs", [1024]); sgu_w_out = din("sgu_w_out", [D, D])
    wr_d = din("wr", [2, D, 20]); rb_d = din("rbias", [40])
    w_gate = din("w_gate", [2, 16, D, 256]); w_up = din("w_up", [2, 16, D, 256]); w_down = din("w_down", [2, 16, 256, D])
    ident_d = din("ident", [128, 128]); sele_d = din("sele", [32, 16 * 128]); invcnt_d = din("invcnt", [128, 64])

    es = ExitStack()
    with es:
        avail = (nc.sbuf_bytes_remaining // 256) * 256 - 256
        arena = es.enter_context(nc.sbuf_tensor("arena", [128, avail // 4], F32))
        psum = es.enter_context(nc.psum_tensor("psum", [128, 4096], F32))
        state = {"off": 0}

        def alloc(shape, dtype=F32, parts=None):
            n = int(np.prod(shape[1:]))
            nbytes = n * (4 if dtype == F32 else 2)
            nbytes = (nbytes + 63) // 64 * 64
            o = state["off"]; state["off"] += nbytes
            assert state["off"] <= avail, ("SBUF overflow", state["off"], avail)
            v = arena[:, o // 4:(o + nbytes) // 4]
            if dtype != F32:
                v = v.bitcast(dtype)
            v = v[:, 0:n]
            if len(shape) == 3:
                v = v.rearrange("p (a b) -> p a b", a=shape[1])
            elif len(shape) == 4:
                v = v.rearrange("p (a b c) -> p a b c", a=shape[1], b=shape[2])
            return v

        def bank(b, n=1):
            return psum[:, b * 512:(b + n) * 512]

        rr = {"b": 0}

        def nextbank():
            b = rr["b"]; rr["b"] = (b + 1) % 8
            return b

        h = alloc([128, 8, TS])
        NSLOT = 6
        ring = [alloc([128, 4096], BF16) for _ in range(NSLOT)]
        ktail = alloc([128, 4, 512], BF16); vtail = alloc([128, 4, 8, 128], BF16); ptail = alloc([128, 4, 16])
        etab = alloc([128, 8, 256])
        ident = alloc([128, 128]); ones_bf = alloc([128, 128], BF16); gains = alloc([128, 5, 8])
        pscale = alloc([128, 4]); b_u = alloc([128, 8]); epst = alloc([128, 1])
        sele = alloc([128, 16, 128], BF16); poolw = alloc([128, 4, 128], BF16); invcnt = alloc([128, 4, 16])
        wr32 = alloc([128, 2, 8, 20]); rbias = alloc([128, 2, 20])
        sqp = [alloc([128, 512], BF16) for _ in range(4)]; rstdp = [alloc([128, 512]) for _ in range(NT)]
        arena_base = state["off"]
        XS_OFF = 57344
        state["off"] = arena_base + XS_OFF
        xs = [alloc([128, D]) for _ in range(4)]
        assert state["off"] <= arena_base + 73728
        state["off"] = arena_base

        ring_state = {"n": 0}

        def ring_load(parts):
            i = ring_state["n"] % NSLOT; ring_state["n"] += 1
            g = P.new_group("ring%d" % i)
            for dst_fn, src in parts:
                dst = dst_fn(ring[i])
                P.add("pool", lambda e, dst=dst, src=src: e.dma_start(out=dst, in_=src), writes=[("ring", i)], grp=g)
            return i

        gc = P.new_group("const")

        gcp = P.new_group("constp")

        def cload(dst, src, eng="sync", key=None):
            P.add(eng, lambda e, dst=dst, src=src: e.dma_start(out=dst, in_=src), writes=[key], grp=(gc if eng == "sync" else gcp))

        cload(ident, ident_d[:, :], key="ident")
        cload(gains.rearrange("p a b -> p (a b)"), gains_d[:, :], key="gains")
        cload(pscale, pscale_d[:, :], key="pscale")
        cload(b_u, bu_d[:, :], key="b_u")
        cload(invcnt.rearrange("p a b -> p (a b)"), invcnt_d[:, :], key="invcnt")
        cload(wr32.rearrange("p l k n -> p l (k n)")[:, 0, :], wr_d[0].rearrange("(p kc) n -> p (kc n)", kc=8), key="wr32")
        cload(wr32.rearrange("p l k n -> p l (k n)")[:, 1, :], wr_d[1].rearrange("(p kc) n -> p (kc n)", kc=8), key="wr32")
        cload(rbias.rearrange("p a b -> p (a b)"), rb_d.partition_broadcast(128), key="rbias")
        cload(sele.rearrange("p a b -> p (a b)")[0:32, :], sele_d[:, :], eng="pool", key="sele")
        cload(poolw, pool_w.rearrange("g c d -> c g d"), eng="pool", key="poolw")
        state["off"] = arena_base
        btab = alloc([128, 8, 256]); r0tab = alloc([128, 8, 256]); emask = alloc([128, 256])
        cload(btab.rearrange("p a b -> p (a b)"), btab_d[:, :], key="btab")
        cload(r0tab.rearrange("p a b -> p (a b)"), r0tab_d[:, :], key="r0tab")
        cload(emask, emask_d[:, :], key="emask")
        P.add("dve", lambda e: e.memset(ones_bf, 1.0), writes=["ones"])
        P.add("dve", lambda e: e.memset(epst, EPS), writes=["eps"])
        P.add("dve", lambda e: e.memset(vtail.rearrange("p a b c -> p (a b c)"), 1.0), writes=["vtail"])
        P.add("dve", lambda e: e.tensor_tensor(out=btab, in0=btab, in1=r0tab, op=ALU.subtract),
              reads=["btab", "r0tab"], writes=["btab"])
        P.add("act", lambda e: e.activation(out=btab, in_=btab, func=AF.Exp), reads=["btab"], writes=["btab"])
        P.add("dve", lambda e: e.tensor_tensor(out=etab, in0=btab, in1=emask.unsqueeze(1).broadcast_to([128, 8, 256]),
                                               op=ALU.mult), reads=["btab", "emask"], writes=["etab"])
        for l in range(2):
            for kc in range(8):
                P.add("dve", lambda e, l=l, kc=kc: e.tensor_scalar(out=wr32[:, l, kc, :], in0=wr32[:, l, kc, :],
                                                                  scalar1=gains[:, 1 + 2 * l, kc:kc + 1], scalar2=None, op0=ALU.mult),
                      reads=["wr32", "gains"], writes=["wr32"])
        P.barrier()

        HK = lambda kc, tt: ("h", kc, tt)
        YK = lambda kc, tt: ("y", kc, tt)

        def norm_stats(tt):
            ts_ = slice(tt * 512, (tt + 1) * 512)
            b = nextbank()
            for kc in range(8):
                P.add("act", lambda e, kc=kc, ts_=ts_: e.activation(out=sqp[kc % 4], in_=h[:, kc, ts_], func=AF.Square),
                      reads=[HK(kc, tt)], writes=[("sq", kc % 4)])
                P.add("pe", lambda e, kc=kc, b=b: e.matmul(bank(b), lhsT=ones_bf, rhs=sqp[kc % 4], start=(kc == 0), stop=(kc == 7)),
                      reads=[("sq", kc % 4), "ones"], writes=[("ps", b)])
            P.add("act", lambda e, b=b, tt=tt: e.activation(out=rstdp[tt], in_=bank(b), func=AF.Ln, scale=1.0 / D, bias=epst[:, 0:1]),
                  reads=[("ps", b), "eps"], writes=[("rstd", tt)])
            P.add("act", lambda e, tt=tt: e.activation(out=rstdp[tt], in_=rstdp[tt], func=AF.Exp, scale=-0.5),
                  reads=[("rstd", tt)], writes=[("rstd", tt)])

        def rmsnorm(gi, ym, out_dtype_note=None):
            for tt in range(NT):
                ts_ = slice(tt * 512, (tt + 1) * 512)
                for kc in range(8):
                    P.add("dve", lambda e, kc=kc, ts_=ts_, tt=tt: e.scalar_tensor_tensor(
                        out=ym[:, kc, ts_], in0=h[:, kc, ts_], scalar=gains[:, gi, kc:kc + 1], in1=rstdp[tt],
                        op0=ALU.mult, op1=ALU.mult), reads=[HK(kc, tt), ("rstd", tt), "gains"], writes=[YK(kc, tt)])
            return rstdp

        def proj_to_h(wslots, src):
            for tt in range(NT):
                ts_ = slice(tt * 512, (tt + 1) * 512)
                for kco in range(8):
                    b = nextbank()
                    for kc in range(8):
                        w = ring[wslots[kc // 4]].rearrange("p (a b) -> p a b", a=4)
                        P.add("pe", lambda e, w=w, kc=kc, kco=kco, b=b, ts_=ts_: e.matmul(
                            bank(b), lhsT=w[:, kc % 4, kco:1024:8], rhs=src[:, kc, ts_], start=(kc == 0), stop=(kc == 7)),
                            reads=[("ring", wslots[kc // 4]), YK(kc, tt)], writes=[("ps", b)])
                    P.add("dve", lambda e, kco=kco, b=b, ts_=ts_: e.tensor_tensor(out=h[:, kco, ts_], in0=bank(b), in1=h[:, kco, ts_], op=ALU.add),
                          reads=[("ps", b), HK(kco, tt)], writes=[HK(kco, tt)])
                norm_stats(tt)

        def load_w_out(wd):
            s = []
            for half in range(2):
                src = wd.rearrange("(kc p) n -> p kc n", p=128)[:, half * 4:(half + 1) * 4, :]
                s.append(ring_load([(lambda r: r.rearrange("p (a b) -> p a b", a=4), src)]))
            return s

        def load_w_in(wd, order=(0, 1, 2, 3)):
            s = [None] * 4
            for q in order:
                src = wd.rearrange("(p kc) n -> p kc n", kc=8)[:, :, q * 512:(q + 1) * 512]
                s[q] = ring_load([(lambda r: r.rearrange("p (a b) -> p a b", a=8), src)])
            return s

        SGU_C_OFF = 73728

        def sgu_const_views():
            o = state["off"]; state["off"] = arena_base + SGU_C_OFF
            v = (alloc([128, 1024]), alloc([128, 8, 128]), alloc([128, 8, 128]), alloc([128, 128]), alloc([128, 2, 8]))
            state["off"] = o
            return v

        def sgu_const_load():
            bvB, bsB, wsT32, trim, lnfm = sgu_const_views()
            gsc = P.new_group("sguc")
            for dst, src, k in ((bvB, bv_d.partition_broadcast(128), "bvB"),
                                (bsB.rearrange("p a b -> p (a b)"), bs_d.partition_broadcast(128), "bsB"),
                                (wsT32.rearrange("p a b -> p (a b)"), wsT_d[:, :], "wsT32"), (trim, trimask_d[:, :], "trim"),
                                (lnfm.rearrange("p a b -> p (a b)"), lnfm_d[:, :], "lnfm")):
                P.add("sync", lambda e, dst=dst, src=src: e.dma_start(out=dst, in_=src), writes=[k], grp=gsc)

        def moe_phase(l):
            state["off"] = arena_base
            if l == 0:
                sgu_const_load()
            tT = alloc([128, 8, TS], BF16)
            rstd = rmsnorm(1 + 2 * l, tT)
            Lb = nextbank(); Tb = nextbank()
            for blk in range(NBLK):
                tt = blk // 4
                for kc in range(8):
                    P.add("pe", lambda e, blk=blk, kc=kc: e.matmul(bank(Lb)[:, blk * 20:(blk + 1) * 20], lhsT=h[:, kc, blk * 128:(blk + 1) * 128],
                                                                  rhs=wr32[:, l, kc, :], start=(kc == 0), stop=(kc == 7)),
                          reads=[HK(kc, tt), "wr32"], writes=[("ps", Lb)])
                P.add("pe", lambda e, blk=blk, tt=tt: e.transpose(bank(Tb)[:, blk * 32:(blk + 1) * 32], rstd[tt][0:32, (blk % 4) * 128:(blk % 4 + 1) * 128], ident[0:32, 0:32]),
                      reads=[("rstd", tt), "ident"], writes=[("ps", Tb)])
            Ls = alloc([128, 8, 20]); rt = alloc([128, 8]); t84 = [alloc([128, 8, 4]) for _ in range(6)]; t8 = [alloc([128, 8]) for _ in range(6)]
            t844 = alloc([128, 8, 4, 4]); chl = alloc([128, 8, 32]); cbf = alloc([128, 8, 16], BF16); cT = alloc([128, TS], BF16)
            R = "rt_scratch"

            def dv(fn, reads=(), writes=()):
                P.add("dve", fn, reads=[R] + list(reads), writes=[R] + list(writes))

            def bc(a):
                return a.unsqueeze(2).broadcast_to([128, 8, 4])

            dv(lambda e: e.tensor_copy(rt, bank(Tb)[:, 0:256:32]), reads=[("ps", Tb)])
            dv(lambda e: e.tensor_tensor(out=Ls, in0=bank(Lb)[:, 0:160].rearrange("p (a b) -> p a b", a=8),
                                         in1=rt.unsqueeze(2).broadcast_to([128, 8, 20]), op=ALU.mult), reads=[("ps", Lb)])
            dv(lambda e: e.tensor_tensor(out=Ls, in0=Ls, in1=rbias[:, l, :].unsqueeze(1).broadcast_to([128, 8, 20]), op=ALU.add), reads=["rbias"])
            gl = Ls[:, :, 0:4]
            gmax, gs, gw, m1, m2, es_ = t8
            oh, gd, esel, e2, sel, en = t84
            dv(lambda e: e.tensor_reduce(out=gmax, in_=gl, axis=AX.X, op=ALU.max))
            dv(lambda e: e.tensor_tensor(out=oh, in0=gl, in1=bc(gmax), op=ALU.is_ge))
            dv(lambda e: e.tensor_tensor(out=gd, in0=gl, in1=bc(gmax), op=ALU.subtract))
            P.add("act", lambda e: e.activation(out=gd, in_=gd, func=AF.Exp), reads=[R], writes=[R])
            dv(lambda e: e.tensor_reduce(out=gs, in_=gd, axis=AX.X, op=ALU.add))
            dv(lambda e: e.reciprocal(out=gw, in_=gs))
            dv(lambda e: e.tensor_tensor(out=t844, in0=Ls[:, :, 4:20].rearrange("p a (g x) -> p a g x", g=4),
                                         in1=oh.unsqueeze(3).broadcast_to([128, 8, 4, 4]), op=ALU.mult))
            dv(lambda e: e.tensor_reduce(out=esel, in_=t844.rearrange("p a g x -> p a x g"), axis=AX.X, op=ALU.add))
            dv(lambda e: e.tensor_reduce(out=m1, in_=esel, axis=AX.X, op=ALU.max))
            dv(lambda e: e.tensor_tensor(out=sel, in0=esel, in1=bc(m1), op=ALU.is_ge))
            dv(lambda e: e.scalar_tensor_tensor(out=e2, in0=sel, scalar=-1e30, in1=esel, op0=ALU.mult, op1=ALU.add))
            dv(lambda e: e.tensor_reduce(out=m2, in_=e2, axis=AX.X, op=ALU.max))
            dv(lambda e: e.tensor_tensor(out=sel, in0=esel, in1=bc(m2), op=ALU.is_ge))
            dv(lambda e: e.tensor_tensor(out=e2, in0=esel, in1=bc(m1), op=ALU.subtract))
            P.add("act", lambda e: e.activation(out=e2, in_=e2, func=AF.Exp), reads=[R], writes=[R])
            dv(lambda e: e.tensor_tensor(out=en, in0=e2, in1=sel, op=ALU.mult))
            dv(lambda e: e.tensor_reduce(out=es_, in_=en, axis=AX.X, op=ALU.add))
            dv(lambda e: e.reciprocal(out=es_, in_=es_))
            dv(lambda e: e.tensor_tensor(out=es_, in0=es_, in1=gw, op=ALU.mult))
            dv(lambda e: e.tensor_tensor(out=en, in0=en, in1=bc(es_), op=ALU.mult))
            dv(lambda e: e.tensor_tensor(out=t844, in0=oh.unsqueeze(3).broadcast_to([128, 8, 4, 4]),
                                         in1=en.unsqueeze(2).broadcast_to([128, 8, 4, 4]), op=ALU.mult))
            comb = t844.rearrange("p a g x -> p a (g x)")
            dv(lambda e: e.tensor_copy(cbf, comb))
            dv(lambda e: e.tensor_copy(chl[:, :, 0:16], cbf))
            dv(lambda e: e.tensor_tensor(out=chl[:, :, 16:32], in0=comb, in1=chl[:, :, 0:16], op=ALU.subtract))
            def emit_cT():
                for half in range(2):
                    cb_ = nextbank()
                    for b4 in range(4):
                        blk = half * 4 + b4
                        P.add("pe", lambda e, blk=blk, b4=b4, cb_=cb_: e.transpose(bank(cb_)[0:32, b4 * 128:(b4 + 1) * 128], chl[:, blk, :], ident),
                              reads=[R, "ident"], writes=[("ps", cb_)])
                    P.add("act", lambda e, half=half, cb_=cb_: e.activation(out=cT[0:32, half * 512:(half + 1) * 512], in_=bank(cb_)[0:32, :], func=AF.Copy),
                          reads=[("ps", cb_)], writes=[("cT", half)])
            ct_done = [False]
            ssb = [alloc([128, 512]) for _ in range(4)]; cbs = [alloc([128, 512]) for _ in range(2)]
            actT = [alloc([128, 4, 512], BF16) for _ in range(2)]
            cnt = {"s": 0, "c": 0, "a": 0}
            slots = {}

            def pair_loads(pi):
                sl = []
                for el in range(2):
                    e_ = 2 * pi + el
                    sl.append(ring_load([
                        (lambda r: r[:, 0:2048], w_gate[l, e_].rearrange("(p kc) f -> p (kc f)", kc=8)),
                        (lambda r: r[:, 2048:4096], w_up[l, e_].rearrange("(p kc) f -> p (kc f)", kc=8))]))
                sd = ring_load([
                    (lambda r: r[:, 0:2048].rearrange("p (a b) -> p a b", a=2), w_down[l, 2 * pi].rearrange("(fc p) d -> p fc d", p=128)),
                    (lambda r: r[:, 2048:4096].rearrange("p (a b) -> p a b", a=2), w_down[l, 2 * pi + 1].rearrange("(fc p) d -> p fc d", p=128))])
                slots[pi] = (sl, sd)

            def emit_E(pi, tt, el):
                sl, sd = slots[pi]
                ts_ = slice(tt * 512, (tt + 1) * 512)
                ai = (pi * NT + tt) % 2
                e_ = 2 * pi + el
                wgu = ring[sl[el]].rearrange("p (g k f) -> p g k f", g=2, k=8)
                ci = cnt["c"] % 2; cnt["c"] += 1
                early = ct_done[0]

                def emit_cb(e_=e_, ci=ci, ts_=ts_, tt=tt):
                    bcb = nextbank()
                    P.add("pe", lambda e, bcb=bcb: e.matmul(bank(bcb), lhsT=sele[0:32, e_, :], rhs=cT[0:32, ts_], start=True, stop=True),
                          reads=["sele", ("cT", tt)], writes=[("ps", bcb)])
                    P.add("act", lambda e, bcb=bcb: e.activation(out=cbs[ci], in_=bank(bcb), func=AF.Copy),
                          reads=[("ps", bcb)], writes=[("cbs", ci)])

                def emit_mults(fc, si, bu_, ci=ci, ai=ai, el=el):
                    P.add("dve", lambda e: e.tensor_tensor(out=ssb[si], in0=ssb[si], in1=cbs[ci], op=ALU.mult),
                          reads=[("ssb", si), ("cbs", ci)], writes=[("ssb", si)])
                    P.add("dve", lambda e: e.tensor_tensor(out=actT[ai][:, el * 2 + fc, :], in0=bank(bu_), in1=ssb[si], op=ALU.mult),
                          reads=[("ps", bu_), ("ssb", si)], writes=[("actT", ai)])

                if early:
                    emit_cb()
                per_fc = []
                for fc in range(2):
                    bg = nextbank(); bu_ = nextbank(); si = cnt["s"] % 4; cnt["s"] += 1
                    for gi_, bb in ((0, bg), (1, bu_)):
                        for kc in range(8):
                            P.add("pe", lambda e, wgu=wgu, gi_=gi_, kc=kc, fc=fc, bb=bb, ts_=ts_: e.matmul(
                                bank(bb), lhsT=wgu[:, gi_, kc, fc * 128:(fc + 1) * 128], rhs=tT[:, kc, ts_], start=(kc == 0), stop=(kc == 7)),
                                reads=[("ring", sl[el]), YK(kc, tt)], writes=[("ps", bb)])
                    P.add("act", lambda e, bg=bg, si=si: e.activation(out=ssb[si], in_=bank(bg), func=AF.Silu),
                          reads=[("ps", bg)], writes=[("ssb", si)])
                    if early:
                        emit_mults(fc, si, bu_)
                    else:
                        per_fc.append((fc, si, bu_))
                if not early:
                    emit_cT(); ct_done[0] = True
                    emit_cb()
                    for fc, si, bu_ in per_fc:
                        emit_mults(fc, si, bu_)

            def emit_D(pi, tt):
                sl, sd = slots[pi]
                ts_ = slice(tt * 512, (tt + 1) * 512)
                ai = (pi * NT + tt) % 2
                for kco in range(8):
                    b = nextbank()
                    for el in range(2):
                        wd = ring[sd][:, el * 2048:(el + 1) * 2048].rearrange("p (a b) -> p a b", a=2)
                        for fc in range(2):
                            first = (el == 0 and fc == 0); last = (el == 1 and fc == 1)
                            P.add("pe", lambda e, wd=wd, fc=fc, kco=kco, b=b, ai=ai, el=el, first=first, last=last: e.matmul(
                                bank(b), lhsT=wd[:, fc, kco:1024:8], rhs=actT[ai][:, el * 2 + fc, :], start=first, stop=last),
                                reads=[("ring", sd), ("actT", ai)], writes=[("ps", b)])
                    P.add("dve", lambda e, kco=kco, b=b, ts_=ts_: e.tensor_tensor(out=h[:, kco, ts_], in0=bank(b), in1=h[:, kco, ts_], op=ALU.add),
                          reads=[("ps", b), HK(kco, tt)], writes=[HK(kco, tt)])
                if pi == 7:
                    norm_stats(tt)

            assert state["off"] <= arena_base + XS_OFF, state["off"] - arena_base
            pts = [(pi, tt) for pi in range(8) for tt in range(NT)]
            pair_loads(0)
            emit_E(0, 0, 0)
            for k, (pi, tt) in enumerate(pts):
                emit_E(pi, tt, 1)
                if k + 1 < len(pts):
                    pn, tn = pts[k + 1]
                    if tn == 0:
                        pair_loads(pn)
                    emit_E(pn, tn, 0)
                emit_D(pi, tt)
            P.barrier()

        out_groups = []
        x_issued = set()

        def issue_x(st_, blk):
            si = blk % 4
            g = P.new_group("xs%d" % si)
            P.add("sync", lambda e, si=si, blk=blk, st_=st_: e.dma_start(out=xs[si], in_=x[st_ * TS + blk * 128:st_ * TS + (blk + 1) * 128, :]),
                  writes=[("xs", si)], grp=g)
            x_issued.add((st_, blk))
        def do_super(st):
            t0 = st * TS
            for blk in range(NBLK):
                si = blk % 4
                if (st, blk) not in x_issued:
                    issue_x(st, blk)
                tt = blk // 4
                for half in range(2):
                    b = nextbank()
                    for k4 in range(4):
                        kc = half * 4 + k4
                        P.add("pe", lambda e, si=si, kc=kc, k4=k4, b=b: e.transpose(bank(b)[:, k4 * 128:(k4 + 1) * 128], xs[si][:, kc:1024:8], ident),
                              reads=[("xs", si), "ident"], writes=[("ps", b)])
                    eng = "dve"
                    dst = h[:, half * 4:(half + 1) * 4, blk * 128:(blk + 1) * 128]
                    src = bank(b).rearrange("p (a b) -> p a b", a=4)
                    if eng == "dve":
                        P.add("dve", lambda e, dst=dst, src=src: e.tensor_copy(dst, src), reads=[("ps", b)], writes=[HK(half * 4 + k, tt) for k in range(4)])
                    else:
                        P.add("act", lambda e, dst=dst, src=src: e.activation(out=dst, in_=src, func=AF.Copy), reads=[("ps", b)], writes=[HK(half * 4 + k, tt) for k in range(4)])
                if blk % 4 == 3:
                    norm_stats(blk // 4)
            P.barrier()

            if stop_stage >= 1:
                state["off"] = arena_base
                ym = alloc([128, 8, TS], BF16)
                rmsnorm(0, ym)
                qT = alloc([128, 4, TS], BF16); kTw = alloc([128, 4, 512 + TS], BF16); Vw = alloc([128, 4 + NBLK, 8, 128], BF16)
                pTg = [alloc([128, 16 + TS]) for _ in range(2)]; tA = alloc([128, 16 + TS]); tB = alloc([128, 16 + TS])
                pooled = alloc([128, TS], BF16); mixP = alloc([128, 4, TS], BF16)
                Pb = [alloc([128, 512], BF16) for _ in range(4)]; Rb = [alloc([128, 128]) for _ in range(2)]
                MK = lambda kc, tt: ("mix", kc, tt)
                if st > 0:
                    P.add("dve", lambda e: e.tensor_copy(kTw[:, :, 0:512], ktail), reads=["ktail"], writes=[("kT", j) for j in range(4)])
                    P.add("dve", lambda e: e.tensor_copy(Vw[:, 0:4].rearrange("p a b c -> p (a b c)"), vtail.rearrange("p a b c -> p (a b c)")),
                          reads=["vtail"], writes=[("V", b_) for b_ in range(4)])
                for b_ in range(NBLK):
                    P.add("dve", lambda e, b_=b_: e.memset(Vw[:, 4 + b_, :, 64:128], 1.0), writes=[("V", 4 + b_)])
                wsl = load_w_in(ab_w_in)
                def proj_qk(which, slot_i):
                    for j in range(4):
                        for tt in range(NT):
                            ts_ = slice(tt * 512, (tt + 1) * 512)
                            b = nextbank()
                            w = ring[wsl[slot_i]].rearrange("p (a b) -> p a b", a=8)
                            for kc in range(8):
                                P.add("pe", lambda e, w=w, kc=kc, j=j, b=b, ts_=ts_: e.matmul(bank(b), lhsT=w[:, kc, j * 128:(j + 1) * 128], rhs=ym[:, kc, ts_],
                                                                                         start=(kc == 0), stop=(kc == 7)),
                                      reads=[("ring", wsl[slot_i]), YK(kc, tt)], writes=[("ps", b)])
                            if which == "q":
                                P.add("act", lambda e, j=j, b=b, ts_=ts_: e.activation(out=qT[:, j, ts_], in_=bank(b), func=AF.Copy),
                                      reads=[("ps", b)], writes=[("qT", j)])
                            else:
                                P.add("dve", lambda e, j=j, b=b, tt=tt: e.tensor_copy(kTw[:, j, 512 + tt * 512:512 + (tt + 1) * 512], bank(b)),
                                      reads=[("ps", b)], writes=[("kT", j)])
                def proj_v(blks):
                    wv = ring[wsl[3]].rearrange("p (a b) -> p a b", a=8)
                    for blk in blks:
                        b = nextbank(); tt = blk // 4
                        for kc in range(8):
                            P.add("pe", lambda e, kc=kc, blk=blk, b=b: e.matmul(bank(b), lhsT=ym[:, kc, blk * 128:(blk + 1) * 128], rhs=wv[:, kc, :],
                                                                          start=(kc == 0), stop=(kc == 7)),
                                  reads=[("ring", wsl[3]), YK(kc, tt)], writes=[("ps", b)])
                        eng = "act" if blk % 2 else "dve"
                        dst = Vw[:, 4 + blk, :, 0:64]; src = bank(b).rearrange("p (a b) -> p a b", a=8)
                        if eng == "dve":
                            P.add("dve", lambda e, dst=dst, src=src: e.tensor_copy(dst, src), reads=[("ps", b)], writes=[("V", 4 + blk)])
                        else:
                            P.add("act", lambda e, dst=dst, src=src: e.activation(out=dst, in_=src, func=AF.Copy), reads=[("ps", b)], writes=[("V", 4 + blk)])
                wp = ring[wsl[0]].rearrange("p (a b) -> p a b", a=8)
                def pool_proj(g_):
                    pt = pTg[g_ % 2]; PK = ("pT", g_ % 2)
                    if st > 0:
                        P.add("dve", lambda e, pt=pt, g_=g_: e.tensor_copy(pt[:, 0:16], ptail[:, g_, :]), reads=["ptail"], writes=[PK])
                    else:
                        P.add("dve", lambda e, pt=pt: e.memset(pt[:, 0:16], 0.0), writes=[PK])
                    for tt in range(NT):
                        ts_ = slice(tt * 512, (tt + 1) * 512)
                        b = nextbank()
                        for kc in range(8):
                            P.add("pe", lambda e, kc=kc, g_=g_, b=b, ts_=ts_: e.matmul(bank(b), lhsT=wp[:, kc, g_ * 128:(g_ + 1) * 128], rhs=ym[:, kc, ts_],
                                                                                 start=(kc == 0), stop=(kc == 7)),
                                  reads=[("ring", wsl[0]), YK(kc, tt)], writes=[("ps", b)])
                        P.add("act", lambda e, pt=pt, b=b, tt=tt: e.activation(out=pt[:, 16 + tt * 512:16 + (tt + 1) * 512], in_=bank(b), func=AF.Copy),
                              reads=[("ps", b)], writes=[PK])
                    P.add("dve", lambda e, pt=pt, g_=g_: e.tensor_copy(ptail[:, g_, :], pt[:, TS:TS + 16]), reads=[PK], writes=["ptail"])
                def pool_chain(g_):
                    pt = pTg[g_ % 2]; PK = ("pT", g_ % 2)
                    W_ = 16 + TS
                    srcb = pt; bufs = [tA, tB]; keys = ["tA", "tB"]; sk = PK
                    for lev in range(g_ + 1):
                        sh = 1 << lev; lo = (1 << (lev + 1)) - 1
                        dstb = bufs[lev % 2]; dk = keys[lev % 2]
                        P.add("dve", lambda e, dstb=dstb, srcb=srcb, sh=sh, lo=lo: e.tensor_tensor(out=dstb[:, lo:W_], in0=srcb[:, lo:W_], in1=srcb[:, lo - sh:W_ - sh], op=ALU.add),
                              reads=[sk], writes=[dk])
                        srcb = dstb; sk = dk
                    wdw = float(1 << (g_ + 1))
                    P.add("dve", lambda e, srcb=srcb, pt=pt, wdw=wdw: e.scalar_tensor_tensor(out=pooled, in0=srcb[:, 16:W_], scalar=1.0 / wdw, in1=pt[:, 16:W_],
                                                                                       op0=ALU.mult, op1=ALU.subtract), reads=[sk, PK], writes=["pooled"])
                    if st == 0:
                        P.add("dve", lambda e, srcb=srcb, g_=g_: e.tensor_tensor(out=srcb[:, 0:16], in0=srcb[:, 16:32], in1=invcnt[:, g_, :], op=ALU.mult),
                              reads=[sk, "invcnt"], writes=[sk])
                        P.add("dve", lambda e, srcb=srcb, pt=pt: e.tensor_tensor(out=pooled[:, 0:16], in0=srcb[:, 0:16], in1=pt[:, 16:32], op=ALU.subtract),
                              reads=[sk, PK, "pooled"], writes=["pooled"])
                def pool_mm(g_):
                    for tt in range(NT):
                        ts_ = slice(tt * 512, (tt + 1) * 512)
                        b = nextbank()
                        P.add("pe", lambda e, g_=g_, b=b, ts_=ts_: e.matmul(bank(b), lhsT=poolw[:, g_, :], rhs=pooled[:, ts_], start=True, stop=True),
                              reads=["poolw", "pooled"], writes=[("ps", b)])
                        P.add("dve", lambda e, g_=g_, b=b, ts_=ts_: e.tensor_scalar(out=mixP[:, g_, ts_], in0=bank(b), scalar1=pscale[:, g_:g_ + 1], scalar2=None, op0=ALU.mult),
                              reads=[("ps", b), "pscale"], writes=[MK(g_, tt)])
                pool_proj(0); pool_proj(1); proj_qk("q", 1); pool_chain(0); pool_mm(0); pool_proj(2); proj_qk("k", 2)
                pool_chain(1); pool_mm(1); pool_proj(3); pool_chain(2); proj_v(range(0, NBLK // 2)); pool_mm(2)
                pool_chain(3); proj_v(range(NBLK // 2, NBLK)); pool_mm(3)
                wo = load_w_out(ab_w_out)
                lbmin = 4 if st == 0 else 0
                LA = 3
                osb = [(tA[:, 0:512], "tA"), (tB[:, 0:512], "tB"), (pTg[0][:, 0:512], ("pT", 0)), (pTg[1][:, 0:512], ("pT", 1))]
                Rn = pooled.bitcast(F32)
                items = []
                for hh in range(8):
                    for half in range(2):
                        mr0 = half * 4; mr1 = half * 4 + 3
                        first = True
                        for lb in range(max(lbmin, mr0), mr1 + 5):
                            m_lo = max(lb - 4, mr0); m_hi = min(lb, mr1)
                            if m_lo <= m_hi:
                                items.append((hh, half, lb, m_lo, m_hi, first)); first = False

                def emit_qk(i):
                    hh, half, lb, m_lo, m_hi, first = items[i]
                    j = hh // 2; po = (hh % 2) * 64
                    nq = (m_hi - m_lo + 1) * 128; q0 = m_lo * 128
                    sb = i % 4
                    P.add("pe", lambda e, j=j, po=po, lb=lb, nq=nq, q0=q0, sb=sb: e.matmul(
                        bank(sb)[:, 0:nq], lhsT=kTw[po:po + 64, j, lb * 128:(lb + 1) * 128], rhs=qT[po:po + 64, j, q0:q0 + nq], start=True, stop=True),
                        reads=[("kT", j), ("qT", j)], writes=[("ps", sb)])

                def emit_rest(i, deferred):
                    hh, half, lb, m_lo, m_hi, first = items[i]
                    mr1_ = half * 4 + 3
                    j = hh // 2; po = (hh % 2) * 64
                    ob = 4 + (hh * 2 + half) % 4
                    OT = bank(ob)
                    if first:
                        P.add("dve", lambda e, OT=OT: e.memset(OT, 0.0), writes=[("ps", ob)])
                    nq = (m_hi - m_lo + 1) * 128
                    sb = i % 4; pi_ = i % 4
                    P.add("act", lambda e, pi_=pi_, sb=sb, nq=nq: e.activation(out=Pb[pi_][:, 0:nq], in_=bank(sb)[:, 0:nq], func=AF.Exp, scale=ATT_SCALE),
                          reads=[("ps", sb)], writes=[("Pb", pi_)])
                    has0 = (m_lo == lb - 4); has1 = (m_lo <= lb - 3 <= m_hi)
                    if has0 and has1:
                        pc, ec = 0, (0, 256)
                    elif has0:
                        pc, ec = 0, (0, 128)
                    elif has1:
                        pc, ec = (lb - 3 - m_lo) * 128, (128, 256)
                    else:
                        pc = None
                    if pc is not None:
                        P.add("dve", lambda e, pi_=pi_, pc=pc, ec=ec, hh=hh: e.tensor_tensor(out=Pb[pi_][:, pc:pc + ec[1] - ec[0]], in0=Pb[pi_][:, pc:pc + ec[1] - ec[0]],
                                                                                       in1=etab[:, hh, ec[0]:ec[1]], op=ALU.mult),
                              reads=[("Pb", pi_), "etab"], writes=[("Pb", pi_)])
                    while deferred and deferred[0][0] <= i - 2:
                        deferred.pop(0)[1]()
                    vfull = Vw[:, lb, hh, :]
                    vhalf = Vw[64:128, lb, hh, :]
                    m = m_lo
                    while m <= m_hi:
                        last = (lb == m + 4); nat = (lb == m)
                        pc0 = (m - m_lo) * 128; oc = (m - half * 4) * 128
                        if nat:
                            P.add("pe", lambda e, oc=oc, pc0=pc0, pi_=pi_, OT=OT, vfull=vfull, last=last: e.matmul(
                                OT[:, oc:oc + 64], lhsT=vfull, rhs=Pb[pi_][:, pc0:pc0 + 64], start=False, stop=last, skip_group_check=True),
                                reads=[("V", lb), ("Pb", pi_)], writes=[("ps", ob)])
                            P.add("pe", lambda e, oc=oc, pc0=pc0, pi_=pi_, OT=OT, vhalf=vhalf, last=last: e.matmul(
                                OT[:, oc + 64:oc + 128], lhsT=vhalf, rhs=Pb[pi_][64:128, pc0 + 64:pc0 + 128], start=False, stop=last, skip_group_check=True),
                                reads=[("V", lb), ("Pb", pi_)], writes=[("ps", ob)])
                            m2_ = m + 1
                        else:
                            m2_ = m + 1
                            while (m2_ <= m_hi and (lb == m2_ + 4) == last and lb != m2_):
                                m2_ += 1
                            nn = (m2_ - m) * 128
                            P.add("pe", lambda e, oc=oc, nn=nn, pc0=pc0, pi_=pi_, OT=OT, vfull=vfull, last=last: e.matmul(
                                OT[:, oc:oc + nn], lhsT=vfull, rhs=Pb[pi_][:, pc0:pc0 + nn], start=False, stop=last, skip_group_check=True),
                                reads=[("V", lb), ("Pb", pi_)], writes=[("ps", ob)])
                        if lb == mr1_ + 4 and m2_ > mr1_:
                            u_ = hh * 2 + half
                            OS, osk = osb[u_ % 4]

                            def norm_(OS=OS, osk=osk, OT=OT, j=j, po=po, ob=ob, half=half):
                                P.add("dve", lambda e: e.tensor_copy(OS, OT), reads=[("ps", ob)], writes=[osk])
                                P.add("act", lambda e: e.activation(out=Rn[0:64, :], in_=OS[64:128, :], func=AF.Ln), reads=[osk], writes=["pooled"])
                                P.add("act", lambda e: e.activation(out=Rn[0:64, :], in_=Rn[0:64, :], func=AF.Exp, scale=-1.0), reads=["pooled"], writes=["pooled"])
                                P.add("dve", lambda e: e.tensor_tensor(out=ym[po:po + 64, j, half * 512:(half + 1) * 512], in0=OS[0:64, :], in1=Rn[0:64, :], op=ALU.mult),
                                      reads=[osk, "pooled"], writes=[YK(j, half)])
                            deferred.append((i, norm_))
                        m = m2_

                for i in range(min(LA, len(items))):
                    emit_qk(i)
                deferred = []
                for i in range(len(items)):
                    if i + LA < len(items):
                        emit_qk(i + LA)
                    emit_rest(i, deferred)
                for _, fn_ in deferred:
                    fn_()
                P.add("dve", lambda e: e.tensor_copy(ktail, kTw[:, :, TS:TS + 512]), reads=[("kT", j) for j in range(4)], writes=["ktail"])
                P.add("dve", lambda e: e.tensor_copy(vtail.rearrange("p a b c -> p (a b c)"), Vw[:, NBLK:NBLK + 4].rearrange("p a b c -> p (a b c)")),
                      reads=[("V", b_) for b_ in range(NBLK, NBLK + 4)], writes=["vtail"])
                for tt in range(NT):
                    ts_ = slice(tt * 512, (tt + 1) * 512)
                    for kco in range(8):
                        b = nextbank()
                        for kc in range(8):
                            w = ring[wo[kc // 4]].rearrange("p (a b) -> p a b", a=4)
                            srcm = mixP[:, kc, ts_] if kc < 4 else ym[:, kc - 4, ts_]
                            P.add("pe", lambda e, w=w, kc=kc, kco=kco, b=b, srcm=srcm: e.matmul(bank(b), lhsT=w[:, kc % 4, kco:1024:8], rhs=srcm,
                                                                                         start=(kc == 0), stop=(kc == 7)),
                                  reads=[("ring", wo[kc // 4]), MK(kc, tt) if kc < 4 else YK(kc - 4, tt)], writes=[("ps", b)])
                        P.add("dve", lambda e, kco=kco, b=b, ts_=ts_: e.tensor_tensor(out=h[:, kco, ts_], in0=bank(b), in1=h[:, kco, ts_], op=ALU.add),
                              reads=[("ps", b), HK(kco, tt)], writes=[HK(kco, tt)])
                    norm_stats(tt)
                P.barrier()
            if stop_stage >= 2:
                moe_phase(0)
            if stop_stage >= 3:
                state["off"] = arena_base
                ym = alloc([128, 8, TS], BF16)
                bvB, bsB, wsT32, trim, lnfm = sgu_const_views()
                Cb = alloc([128, 8, 128]); wsT = alloc([128, 8, 128], BF16)
                P.add("dve", lambda e: e.tensor_tensor(out=wsT, in0=wsT32, in1=trim.unsqueeze(1).broadcast_to([128, 8, 128]), op=ALU.mult),
                      reads=["wsT32", "trim"], writes=["wsT"])
                for half in range(2):
                    b = nextbank()
                    P.add("pe", lambda e, half=half, b=b: e.matmul(bank(b), lhsT=ones_bf, rhs=wsT.rearrange("p a b -> p (a b)")[:, half * 512:(half + 1) * 512], start=True, stop=True),
                          reads=["wsT", "ones"], writes=[("ps", b)])
                    for h4 in range(4):
                        hh = half * 4 + h4
                        P.add("dve", lambda e, hh=hh, h4=h4, b=b: e.scalar_tensor_tensor(out=Cb[:, hh, :], in0=bank(b)[:, h4 * 128:(h4 + 1) * 128], scalar=lnfm[:, 1, hh:hh + 1],
                                                                                   in1=bsB[:, hh, :], op0=ALU.mult, op1=ALU.add),
                              reads=[("ps", b), "lnfm", "bsB"], writes=["Cb"])
                rmsnorm(2, ym)
                uT = alloc([128, 8, TS], BF16); vn = alloc([128, NBLK, 1024], BF16)
                vpre = [alloc([128, 1024]) for _ in range(2)]; tmp5 = [alloc([128, 512]) for _ in range(2)]
                stats = alloc([128, NBLK, 2, 6]); mv = alloc([128, NBLK, 2]); lrs = alloc([128, NBLK]); nmean = alloc([128, NBLK])
                assert state["off"] <= arena_base + SGU_C_OFF, state["off"] - arena_base
                wsl = load_w_in(sgu_w_in, order=(2, 3, 0, 1))
                for blk in range(NBLK):
                    tt = blk // 4; vi = blk % 2
                    for half in range(2):
                        w = ring[wsl[2 + half]].rearrange("p (a b) -> p a b", a=8)
                        b = nextbank()
                        for kc in range(8):
                            P.add("pe", lambda e, w=w, kc=kc, blk=blk, b=b: e.matmul(bank(b), lhsT=ym[:, kc, blk * 128:(blk + 1) * 128], rhs=w[:, kc, :],
                                                                               start=(kc == 0), stop=(kc == 7)),
                                  reads=[("ring", wsl[2 + half]), YK(kc, tt)], writes=[("ps", b)])
                        P.add("dve", lambda e, vi=vi, half=half, b=b: e.tensor_tensor(out=vpre[vi][:, half * 512:(half + 1) * 512], in0=bank(b),
                                                                                  in1=bvB[:, half * 512:(half + 1) * 512], op=ALU.add),
                              reads=[("ps", b), "bvB"], writes=[("vpre", vi)])
                    P.add("act", lambda e, vi=vi, blk=blk: e.activation(out=vn[:, blk, :], in_=vpre[vi], func=AF.Gelu), reads=[("vpre", vi)], writes=[("vn", blk)])
                    for half in range(2):
                        P.add("dve", lambda e, half=half, blk=blk: e.bn_stats(out=stats[:, blk, half, :], in_=vn[:, blk, half * 512:(half + 1) * 512]),
                              reads=[("vn", blk)], writes=["stats"])
                    P.add("dve", lambda e, blk=blk: e.bn_aggr(out=mv[:, blk, :], in_=stats[:, blk, :, :]), reads=["stats"], writes=["mv"])
                for c in range(8):
                    w = ring[wsl[c // 4]].rearrange("p (a b) -> p a b", a=8)
                    for tt in range(NT):
                        ts_ = slice(tt * 512, (tt + 1) * 512)
                        b = nextbank()
                        for kc in range(8):
                            P.add("pe", lambda e, w=w, kc=kc, c=c, b=b, ts_=ts_: e.matmul(bank(b), lhsT=w[:, kc, (c % 4) * 128:(c % 4 + 1) * 128], rhs=ym[:, kc, ts_],
                                                                                     start=(kc == 0), stop=(kc == 7)),
                                  reads=[("ring", wsl[c // 4]), YK(kc, tt)], writes=[("ps", b)])
                        P.add("act", lambda e, c=c, b=b, ts_=ts_: e.activation(out=uT[:, c, ts_], in_=bank(b), func=AF.Gelu, bias=b_u[:, c:c + 1]),
                              reads=[("ps", b), "b_u"], writes=[("uT", c, tt)])
                P.add("act", lambda e: e.activation(out=lrs, in_=mv[:, :, 1], func=AF.Ln, bias=epst[:, 0:1]), reads=["mv", "eps"], writes=["lrs"])
                P.add("act", lambda e: e.activation(out=lrs, in_=lrs, func=AF.Exp, scale=-0.5), reads=["lrs"], writes=["lrs"])
                wo = load_w_out(sgu_w_out)
                P.add("dve", lambda e: e.scalar_tensor_tensor(out=nmean, in0=mv[:, :, 0], scalar=-1.0, in1=lrs, op0=ALU.mult, op1=ALU.mult),
                      reads=["mv", "lrs"], writes=["nmean"])
                for blk in range(NBLK):
                    P.add("act", lambda e, blk=blk: e.activation(out=vn[:, blk, :], in_=vn[:, blk, :], func=AF.Identity, scale=lrs[:, blk:blk + 1], bias=nmean[:, blk:blk + 1]),
                          reads=[("vn", blk), "nmean", "lrs"], writes=[("vn", blk)])
                for half in range(NBLK // 4):
                    for hh in range(8):
                        b = nextbank(); ti = hh % 2
                        for b4 in range(4):
                            blk = half * 4 + b4
                            P.add("pe", lambda e, hh=hh, blk=blk, b4=b4, b=b: e.matmul(bank(b)[:, b4 * 128:(b4 + 1) * 128], lhsT=vn[:, blk, hh * 128:(hh + 1) * 128],
                                                                                  rhs=wsT[:, hh, :], start=True, stop=True),
                                  reads=[("vn", blk), "wsT"], writes=[("ps", b)])
                        P.add("dve", lambda e, hh=hh, b=b, ti=ti: e.scalar_tensor_tensor(out=tmp5[ti].rearrange("p (a b) -> p a b", a=4), in0=bank(b).rearrange("p (a b) -> p a b", a=4),
                                                                                    scalar=lnfm[:, 0, hh:hh + 1], in1=Cb[:, hh, :].unsqueeze(1).broadcast_to([128, 4, 128]),
                                                                                    op0=ALU.mult, op1=ALU.add),
                              reads=[("ps", b), "Cb", "lnfm"], writes=[("tmp5", ti)])
                        P.add("dve", lambda e, hh=hh, half=half, ti=ti: e.tensor_tensor(out=ym[:, hh, half * 512:(half + 1) * 512], in0=tmp5[ti],
                                                                                   in1=uT[:, hh, half * 512:(half + 1) * 512], op=ALU.mult),
                              reads=[("tmp5", ti), ("uT", hh, half)], writes=[YK(hh, half)])
                proj_to_h(wo, ym)
                P.barrier()
            if stop_stage >= 4:
                moe_phase(1)
            state["off"] = arena_base
            yf = alloc([128, 8, TS])
            if st + 1 < n_super:
                issue_x(st + 1, 0); issue_x(st + 1, 1); issue_x(st + 1, 2); issue_x(st + 1, 3)
            if stop_stage >= 5:
                rmsnorm(4, yf)
                src_t = yf; SK = YK
            else:
                src_t = h; SK = HK
            osl = [alloc([128, D]) for _ in range(4)]
            assert state["off"] <= arena_base + XS_OFF, state["off"] - arena_base
            for blk in range(NBLK):
                si = blk % 4; tt = blk // 4
                ov = osl[si].rearrange("t (p kc) -> t kc p", kc=8)
                for half in range(2):
                    b = nextbank()
                    for k4 in range(4):
                        kc = half * 4 + k4
                        P.add("pe", lambda e, kc=kc, k4=k4, b=b, blk=blk: e.transpose(bank(b)[:, k4 * 128:(k4 + 1) * 128], src_t[:, kc, blk * 128:(blk + 1) * 128], ident),
                              reads=[SK(kc, tt), "ident"], writes=[("ps", b)])
                    dst = ov[:, half * 4:(half + 1) * 4, :]; src = bank(b).rearrange("p (a b) -> p a b", a=4)
                    P.add("act", lambda e, dst=dst, src=src: e.activation(out=dst, in_=src, func=AF.Copy), reads=[("ps", b)], writes=[("os", si)])
                g = P.new_group("os%d" % si)
                P.add("sync", lambda e, si=si, blk=blk, t0=t0: e.dma_start(out=out[t0 + blk * 128:t0 + (blk + 1) * 128, :], in_=osl[si]),
                      reads=[("os", si)], writes=[], grp=g)
                out_groups.append(g)
            if st == n_super - 1:
                P.barrier()
        for st_ in range(n_super):
            do_super(st_)
        P.final_tokens = out_groups[-4:]
        P.emit(nc, es)
    return nc


def _host_consts(inp):
    c = {}
    f = lambda a: np.ascontiguousarray(a, dtype=np.float32)
    gains = np.stack([inp["norm_mix_g"][0], inp["norm_ffn_g"][0], inp["norm_mix_g"][1], inp["norm_ffn_g"][1], inp["final_norm_g"]], 0)
    c["gains"] = f(gains.reshape(5, 128, 8).transpose(1, 0, 2).reshape(128, 40))
    c["ab_w_in"] = f(inp["ab_w_in"][0]); c["pool_w"] = f(inp["pool_w"][0])
    c["pscale"] = f(inp["pool_scale"][0].reshape(4, 128).T)
    rb = inp["att_rel_bias"][0]
    p = np.arange(128)[:, None, None]; i = np.arange(64)[None, None, :]
    order = [0, 1, 2, 3]
    idx = np.concatenate([np.clip(p - 64 * d - i, -128, 128) + 128 for d in order], axis=2)
    idx = np.broadcast_to(idx, (128, 8, 256))
    hh = np.arange(8)[None, :, None]
    c["btab"] = f(rb[hh, idx].reshape(128, 2048))
    c["r0tab"] = f(np.broadcast_to(rb[:, 0][None, :, None], (128, 8, 256)).reshape(128, 2048))
    em = np.ones((128, 256), np.float32); em[64:, 0:64] = 0.0
    c["emask"] = em
    c["ab_w_out"] = f(inp["ab_w_out"][0]); c["sgu_w_in"] = f(inp["sgu_w_in"][0])
    c["bu"] = f(inp["sgu_b_in"][0][:1024].reshape(8, 128).T); c["bv"] = f(inp["sgu_b_in"][0][1024:])
    c["lnfm"] = f(np.stack([inp["sgu_ln_g"][0].reshape(8, 128).T, inp["sgu_ln_b"][0].reshape(8, 128).T], axis=1).reshape(128, 16))
    c["wsT"] = f(inp["sgu_w_s"][0].transpose(2, 0, 1).reshape(128, 1024))
    c["trimask"] = f(np.triu(np.ones((128, 128), np.float32)))
    c["bs"] = f(inp["sgu_b_s"][0].reshape(1024)); c["sgu_w_out"] = f(inp["sgu_w_out"][0])
    wr = [np.concatenate([inp["moe_wg_router"][l], inp["moe_we_router"][l].transpose(1, 0, 2).reshape(D, 16)], axis=1) for l in range(2)]
    c["wr"] = f(np.stack(wr, 0))
    c["rbias"] = f(np.concatenate([np.concatenate([inp["moe_bg_router"][l], inp["moe_be_router"][l].reshape(16)]) for l in range(2)]))
    c["w_gate"] = f(inp["moe_w_gate"]); c["w_up"] = f(inp["moe_w_up"]); c["w_down"] = f(inp["moe_w_down"])
    c["ident"] = np.eye(128, dtype=np.float32)
    se = np.zeros((32, 16, 128), np.float32)
    for e in range(16):
        se[e, e, :] = 1.0; se[16 + e, e, :] = 1.0
    c["sele"] = se.reshape(32, 2048)
    ic = np.zeros((128, 4, 16), np.float32)
    for g in range(4):
        w = 2 ** (g + 1)
        ic[:, g, :] = 1.0 / np.minimum(np.arange(16) + 1, w)
    c["invcnt"] = ic.reshape(128, 64)
    return c


_NC_CACHE = {}


def kernel(**inputs):
    inp = {k: np.asarray(v) for k, v in inputs.items()}
    consts = _host_consts(inp)
    if "nc" not in _NC_CACHE:
        _NC_CACHE["nc"] = build_program()
    nc = _NC_CACHE["nc"]
    xin = np.ascontiguousarray(inp["x"], dtype=np.float32)
    in_maps = []
    for b in range(8):
        m = dict(consts); m["x"] = xin[b]
        in_maps.append(m)
    res = run_bass_kernel_spmd(nc, in_maps, core_ids=list(range(8)))
    return np.stack([np.asarray(r["out"], dtype=np.float32) for r in res.results], axis=0)
```
